# Optimizing a Trainium2 kernel written in Bass

```python
import jax
import jax.numpy as jnp
from jax import lax
import numpy as np

D_MODEL = 4096
BATCH = 4
SEQ = 4096
DEPTH = 1

HEAD_DIM = 128
ATTN_HEADS = D_MODEL // HEAD_DIM
ATTN_KV_HEADS = ATTN_HEADS // 4
Q_WIDTH = ATTN_HEADS * HEAD_DIM
KV_WIDTH = ATTN_KV_HEADS * HEAD_DIM
WINDOW = 128
BLOCK = 128
ATTN_SCALE = HEAD_DIM ** -0.5
ROPE_DIM = HEAD_DIM // 4
ROPE_THETA = 500000.0
LRU_WIDTH = D_MODEL
LRU_BLOCK_DIM = 256
LRU_BLOCKS = LRU_WIDTH // LRU_BLOCK_DIM
CONV_WIDTH = 4
LRU_C = 8.0
MEM_LEN = 256
MEM_HEADS = 4
MEM_HEAD_DIM = 128
MEM_WIDTH = MEM_HEADS * MEM_HEAD_DIM
MEM_SCALE = MEM_HEAD_DIM ** -0.5
N_EXPERTS = 32
TOP_K = 4
EXPERT_DIM = (3 * D_MODEL) // 8
SWIGLU_LIMIT = 7.0
SWIGLU_ALPHA = 1.702
LN_EPS = 1e-5
DEEPNORM_ALPHA = (2.0 * DEPTH) ** 0.25
DEEPNORM_BETA = (8.0 * DEPTH) ** -0.25
IN_SPLITS = (Q_WIDTH, KV_WIDTH, KV_WIDTH, LRU_WIDTH, LRU_WIDTH, D_MODEL, D_MODEL)
IN_WIDTH = sum(IN_SPLITS)

kernel_name = 'hybrid_swa_rglru_memxattn_moe_layer'


def split_columns(h, sizes):
    idx = []
    acc = 0
    for s in sizes[:-1]:
        acc += s
        idx.append(acc)
    return jnp.split(h, idx, axis=-1)


def layer_norm(x, g, b):
    xf = x.astype(jnp.float32)
    mu = jnp.mean(xf, axis=-1, keepdims=True)
    var = jnp.mean(jnp.square(xf - mu), axis=-1, keepdims=True)
    y = (xf - mu) * lax.rsqrt(var + LN_EPS) * g.astype(jnp.float32) + b.astype(jnp.float32)
    return y.astype(x.dtype)


def partial_rope(t, positions):
    inv_freq = 1.0 / (ROPE_THETA ** (jnp.arange(0, ROPE_DIM, 2, dtype=jnp.float32) / ROPE_DIM))
    ang = positions.astype(jnp.float32)[..., None] * inv_freq
    cos = jnp.cos(ang)[:, :, None, :]
    sin = jnp.sin(ang)[:, :, None, :]
    tr = t[..., :ROPE_DIM].astype(jnp.float32)
    t1, t2 = tr[..., :ROPE_DIM // 2], tr[..., ROPE_DIM // 2:]
    rot = jnp.concatenate([t1 * cos - t2 * sin, t2 * cos + t1 * sin], axis=-1)
    return jnp.concatenate([rot.astype(t.dtype), t[..., ROPE_DIM:]], axis=-1)


def sliding_window_attention(q, k, v, sinks):
    B, S, Hq, Dh = q.shape
    Hkv = k.shape[2]
    G = Hq // Hkv
    nb = S // BLOCK
    qb = q.reshape(B, nb, BLOCK, Hkv, G, Dh)

    def with_prev(t):
        tb = t.reshape(B, nb, BLOCK, Hkv, Dh)
        prev = jnp.pad(tb[:, :-1], ((0, 0), (1, 0), (0, 0), (0, 0), (0, 0)))
        return jnp.concatenate([prev, tb], axis=2)

    kb = with_prev(k)
    vb = with_prev(v)
    scores = jnp.einsum('bnqhgd,bnshd->bnhgqs', qb, kb,
                        preferred_element_type=jnp.float32) * ATTN_SCALE
    qi = jnp.arange(BLOCK)[:, None]
    si = jnp.arange(2 * BLOCK)[None, :]
    delta = BLOCK + qi - si
    band = (delta >= 0) & (delta < WINDOW)
    blk = jnp.arange(nb)[:, None, None]
    mask = band[None] & ((blk > 0) | (si >= BLOCK)[None])
    scores = jnp.where(mask[None, :, None, None], scores, -jnp.inf)
    sink = sinks.astype(jnp.float32).reshape(Hkv, G)[None, None, :, :, None, None]
    m = jnp.maximum(jnp.max(scores, axis=-1, keepdims=True), sink)
    p = jnp.exp(scores - m)
    probs = p / (jnp.sum(p, axis=-1, keepdims=True) + jnp.exp(sink - m))
    out = jnp.einsum('bnhgqs,bnshd->bnqhgd', probs.astype(v.dtype), vb)
    return out.reshape(B, S, Hq * Dh)


def rglru_branch(xl, yl, conv_w, conv_b, w_ra, b_ra, w_ri, b_ri, lam):
    B, S, C = xl.shape
    xc = lax.conv_general_dilated(xl, conv_w[:, None, :], window_strides=(1,),
                                  padding=[(CONV_WIDTH - 1, 0)],
                                  dimension_numbers=('NWC', 'WIO', 'NWC'),
                                  feature_group_count=C) + conv_b
    xb = xc.reshape(B, S, LRU_BLOCKS, LRU_BLOCK_DIM)
    r = jax.nn.sigmoid(jnp.einsum('bshi,hij->bshj', xb, w_ra) + b_ra).reshape(B, S, C)
    i = jax.nn.sigmoid(jnp.einsum('bshi,hij->bshj', xb, w_ri) + b_ri).reshape(B, S, C)
    log_a = -LRU_C * r.astype(jnp.float32) * jax.nn.softplus(-lam.astype(jnp.float32))
    a = jnp.exp(log_a)
    gated_x = jnp.sqrt(-jnp.expm1(2.0 * log_a)) * (i * xc).astype(jnp.float32)

    def combine(left, right):
        a1, b1 = left
        a2, b2 = right
        return a1 * a2, a2 * b1 + b2

    _, h = lax.associative_scan(combine, (a, gated_x), axis=1)
    return h.astype(xl.dtype) * jax.nn.gelu(yl)


def memory_cross_attention(x, mem, w_q, w_kv, w_o):
    B, S, _ = x.shape
    M = mem.shape[1]
    q = (x @ w_q).reshape(B, S, MEM_HEADS, MEM_HEAD_DIM)
    k, v = jnp.split(mem @ w_kv, 2, axis=-1)
    k = k.reshape(B, M, MEM_HEADS, MEM_HEAD_DIM)
    v = v.reshape(B, M, MEM_HEADS, MEM_HEAD_DIM)
    s = jnp.einsum('bshd,bmhd->bhsm', q, k, preferred_element_type=jnp.float32) * MEM_SCALE
    p = jax.nn.softmax(s, axis=-1).astype(v.dtype)
    o = jnp.einsum('bhsm,bmhd->bshd', p, v).reshape(B, S, MEM_WIDTH)
    return o @ w_o


def moe_ffn(x, w_router, b_router, w_gate_up, b_gate_up, w_down, b_down):
    B, S, D = x.shape
    xf = x.reshape(B * S, D)
    logits = (xf @ w_router).astype(jnp.float32) + b_router.astype(jnp.float32)
    top_vals, top_idx = lax.top_k(logits, TOP_K)
    top_w = jax.nn.softmax(top_vals, axis=-1)
    combine_w = jnp.sum(jax.nn.one_hot(top_idx, N_EXPERTS, dtype=jnp.float32)
                        * top_w[..., None], axis=1)

    def expert_step(acc, ew):
        wgu, bgu, wd, bd, cw = ew
        h = xf @ wgu + bgu
        gate = jnp.minimum(h[:, :EXPERT_DIM], SWIGLU_LIMIT)
        up = jnp.clip(h[:, EXPERT_DIM:], -SWIGLU_LIMIT, SWIGLU_LIMIT)
        glu = gate * jax.nn.sigmoid(SWIGLU_ALPHA * gate)
        out = ((up + 1.0) * glu) @ wd + bd
        return acc + cw[:, None] * out.astype(jnp.float32), None

    acc0 = jnp.zeros((B * S, D), jnp.float32)
    acc, _ = lax.scan(expert_step, acc0, (w_gate_up, b_gate_up, w_down, b_down, combine_w.T))
    return acc.astype(x.dtype).reshape(B, S, D)


def setup_inputs(seed: int = 0) -> dict:
    key = jax.random.key(seed)
    k = jax.random.split(key, 32)

    def nrm(kk, shape, scale):
        return jax.random.normal(kk, shape, jnp.float32) * scale

    L = DEPTH
    a0 = jax.random.uniform(k[9], (L, LRU_WIDTH), jnp.float32, minval=0.9, maxval=0.999)
    offsets = jax.random.randint(k[2], (BATCH, 1), 0, 1024, dtype=jnp.int32)
    return {
        'x': nrm(k[0], (BATCH, SEQ, D_MODEL), 1.0),
        'mem': nrm(k[1], (BATCH, MEM_LEN, D_MODEL), 1.0),
        'positions': offsets + jnp.arange(SEQ, dtype=jnp.int32)[None, :],
        'w_in': nrm(k[3], (L, D_MODEL, IN_WIDTH), D_MODEL ** -0.5),
        'b_gate': nrm(k[4], (L, 2, D_MODEL), 0.02),
        'attn_sinks': nrm(k[5], (L, ATTN_HEADS), 0.5),
        'conv_w': nrm(k[6], (L, CONV_WIDTH, LRU_WIDTH), CONV_WIDTH ** -0.5),
        'conv_b': nrm(k[7], (L, LRU_WIDTH), 0.02),
        'w_lru_a': nrm(k[8], (L, LRU_BLOCKS, LRU_BLOCK_DIM, LRU_BLOCK_DIM), LRU_BLOCK_DIM ** -0.5),
        'b_lru_a': nrm(k[10], (L, LRU_BLOCKS, LRU_BLOCK_DIM), 0.02),
        'w_lru_i': nrm(k[11], (L, LRU_BLOCKS, LRU_BLOCK_DIM, LRU_BLOCK_DIM), LRU_BLOCK_DIM ** -0.5),
        'b_lru_i': nrm(k[12], (L, LRU_BLOCKS, LRU_BLOCK_DIM), 0.02),
        'lru_lambda': jnp.log(a0) - jnp.log1p(-a0),
        'w_branch_attn': nrm(k[13], (L, Q_WIDTH, D_MODEL), DEEPNORM_BETA * Q_WIDTH ** -0.5),
        'w_branch_lru': nrm(k[14], (L, LRU_WIDTH, D_MODEL), DEEPNORM_BETA * LRU_WIDTH ** -0.5),
        'w_mix_out': nrm(k[15], (L, D_MODEL, D_MODEL), DEEPNORM_BETA * D_MODEL ** -0.5),
        'ln1_g': 1.0 + nrm(k[16], (L, D_MODEL), 0.02),
        'ln1_b': nrm(k[17], (L, D_MODEL), 0.02),
        'w_mem_q': nrm(k[18], (L, D_MODEL, MEM_WIDTH), D_MODEL ** -0.5),
        'w_mem_kv': nrm(k[19], (L, D_MODEL, 2 * MEM_WIDTH), D_MODEL ** -0.5),
        'w_mem_o': nrm(k[20], (L, MEM_WIDTH, D_MODEL), DEEPNORM_BETA * MEM_WIDTH ** -0.5),
        'ln2_g': 1.0 + nrm(k[21], (L, D_MODEL), 0.02),
        'ln2_b': nrm(k[22], (L, D_MODEL), 0.02),
        'w_router': nrm(k[23], (L, D_MODEL, N_EXPERTS), D_MODEL ** -0.5),
        'b_router': nrm(k[24], (L, N_EXPERTS), 0.01),
        'w_gate_up': nrm(k[25], (L, N_EXPERTS, D_MODEL, 2 * EXPERT_DIM), D_MODEL ** -0.5),
        'b_gate_up': nrm(k[26], (L, N_EXPERTS, 2 * EXPERT_DIM), 0.02),
        'w_down': nrm(k[27], (L, N_EXPERTS, EXPERT_DIM, D_MODEL), DEEPNORM_BETA * EXPERT_DIM ** -0.5),
        'b_down': nrm(k[28], (L, N_EXPERTS, D_MODEL), 0.02),
        'ln3_g': 1.0 + nrm(k[29], (L, D_MODEL), 0.02),
        'ln3_b': nrm(k[30], (L, D_MODEL), 0.02),
    }


def reference(x, mem, positions, w_in, b_gate, attn_sinks, conv_w, conv_b, w_lru_a, b_lru_a,
              w_lru_i, b_lru_i, lru_lambda, w_branch_attn, w_branch_lru, w_mix_out, ln1_g, ln1_b,
              w_mem_q, w_mem_kv, w_mem_o, ln2_g, ln2_b, w_router, b_router, w_gate_up, b_gate_up,
              w_down, b_down, ln3_g, ln3_b):
    B, S, _ = x.shape
    for l in range(DEPTH):
        h = x @ w_in[l]
        q, k, v, xl, yl, ga, gl = split_columns(h, IN_SPLITS)
        q = partial_rope(q.reshape(B, S, ATTN_HEADS, HEAD_DIM), positions)
        k = partial_rope(k.reshape(B, S, ATTN_KV_HEADS, HEAD_DIM), positions)
        v = v.reshape(B, S, ATTN_KV_HEADS, HEAD_DIM)
        o_attn = sliding_window_attention(q, k, v, attn_sinks[l])
        o_lru = rglru_branch(xl, yl, conv_w[l], conv_b[l], w_lru_a[l], b_lru_a[l],
                             w_lru_i[l], b_lru_i[l], lru_lambda[l])
        g_attn = jax.nn.sigmoid(ga + b_gate[l, 0])
        g_lru = jax.nn.sigmoid(gl + b_gate[l, 1])
        mixed = g_attn * (o_attn @ w_branch_attn[l]) + g_lru * (o_lru @ w_branch_lru[l])
        x = layer_norm(DEEPNORM_ALPHA * x + mixed @ w_mix_out[l], ln1_g[l], ln1_b[l])
        xa = memory_cross_attention(x, mem, w_mem_q[l], w_mem_kv[l], w_mem_o[l])
        x = layer_norm(DEEPNORM_ALPHA * x + xa, ln2_g[l], ln2_b[l])
        ff = moe_ffn(x, w_router[l], b_router[l], w_gate_up[l], b_gate_up[l], w_down[l], b_down[l])
        x = layer_norm(DEEPNORM_ALPHA * x + ff, ln3_g[l], ln3_b[l])
    return x
```

```python
import math
import numpy as np
import concourse.bass as bass
import concourse.mybir as mybir
from concourse.bass_utils import run_bass_kernel_spmd

F32 = mybir.dt.float32
BF16 = mybir.dt.bfloat16
I32 = mybir.dt.int32
AF = mybir.ActivationFunctionType
ALU = mybir.AluOpType
AX = mybir.AxisListType

D = 4096
NB = 512
ALPHA = 2.0 ** 0.25
EPS = 1e-5
SCALE = 128.0 ** -0.5
NEG = -30000.0
ENGS = ['pe', 'act', 'dve', 'pool', 'sp']
NDS = 8


class Tile:
    __slots__ = ("w", "r", "psum")

    def __init__(self):
        self.w = {}
        self.r = {}
        self.psum = False


class Prog:
    def __init__(self):
        self.q = {e: [] for e in ENGS}
        self.ms = {e: 0 for e in ENGS}
        self.waited = {}
        self.dcnt = {(qe, j): 0 for qe in ('sp', 'pool') for j in range(NDS)}
        self.drr = {'sp': 0, 'pool': 0}

    def _collect(self, eng, reads, writes):
        need = {}
        for t in reads:
            for k, v in t.w.items():
                if need.get(k, 0) < v:
                    need[k] = v
        for t in writes:
            for k, v in t.w.items():
                if need.get(k, 0) < v:
                    need[k] = v
            for k, v in t.r.items():
                if need.get(k, 0) < v:
                    need[k] = v
        waits = []
        for k, v in need.items():
            if k == eng and eng == 'pe':
                continue
            if self.waited.get((eng, k), 0) < v:
                self.waited[(eng, k)] = v
                waits.append((k, v))
        return waits

    def _mark(self, tok, reads, writes):
        k, v = tok
        for t in reads:
            if t.r.get(k, 0) < v:
                t.r[k] = v
        for t in writes:
            if t.w.get(k, 0) < v:
                t.w[k] = v

    def op(self, eng, fn, reads=(), writes=()):
        if eng != 'pe':
            ex = [t for t in reads if getattr(t, 'psum', False)]
            if ex:
                writes = list(writes) + ex
        waits = self._collect(eng, reads, writes)
        self.ms[eng] += 1
        tok = (eng, self.ms[eng])
        self.q[eng].append((fn, waits, eng, 1))
        self._mark(tok, reads, writes)

    def dma(self, qe, fn, reads=(), writes=()):
        j = self.drr[qe]
        self.drr[qe] = (j + 1) % NDS
        key = (qe, j)
        waits = self._collect(qe, reads, writes)
        prev = 16 * self.dcnt[key]
        if prev > 0 and self.waited.get((qe, key), 0) < prev:
            self.waited[(qe, key)] = prev
            waits.append((key, prev))
        self.dcnt[key] += 1
        tok = (key, 16 * self.dcnt[key])
        self.q[qe].append((fn, waits, key, 16))
        self._mark(tok, reads, writes)

    def fence(self):
        for e in ENGS:
            waits = []
            for k in ENGS:
                if k == e:
                    continue
                v = self.ms[k]
                if v > 0 and self.waited.get((e, k), 0) < v:
                    self.waited[(e, k)] = v
                    waits.append((k, v))
            for key, c in self.dcnt.items():
                v = 16 * c
                if v > 0 and self.waited.get((e, key), 0) < v:
                    self.waited[(e, key)] = v
                    waits.append((key, v))
            if waits:
                self.q[e].append((None, waits, None, 0))


_DBGSW = {}


class _Stop(Exception):
    pass


def build_program(blocks=(-4, -3, -2, -1, 0, 1, 2, 3), nexp=32, dbg=None, last=None):
    nc = bass.Bass("TRN2", target_bir_lowering=False)

    decl = {}
    need_lvl = {"w_in": 3, "w_ba": 5, "w_bl": 7, "w_mo": 8, "w_mq": 9, "w_mout": 9, "w_gu": 11, "w_dn": 11,
                "w_lru_a": 6, "w_lru_i": 6, "w_mkv": 1}

    def din(name, shape, dt=F32):
        if last is not None and name in need_lvl and need_lvl[name] > last:
            shape = [128, 128]
        decl[name] = list(shape)
        return nc.dram_tensor(name, shape, dt, kind="ExternalInput").ap()

    xc = din("xc", [4096, D])
    memc = din("memc", [256, D])
    posc = din("posc", [1, 2176], I32)
    flag = din("flag", [128, 1])
    masks = din("masks", [3, 128, 512])
    identd = din("ident", [128, 128])
    rotmd = din("rotm", [32, 32])
    invfd = din("invf", [32, 1])
    w_in = din("w_in", [D, 22528])
    vecs = din("vecs", [512, 128])
    sinks = din("sinks", [1, 32])
    w_lru_a = din("w_lru_a", [16, 256, 256])
    w_lru_i = din("w_lru_i", [16, 256, 256])
    w_ba = din("w_ba", [D, D])
    w_bl = din("w_bl", [D, D])
    w_mo = din("w_mo", [D, D])
    w_mq = din("w_mq", [D, 512])
    w_mkv = din("w_mkv", [D, 1024])
    w_mout = din("w_mout", [512, D])
    w_r = din("w_r", [D, 32])
    b_r = din("b_r", [1, 32])
    w_gu = din("w_gu", [nexp, D, 3072])
    b_gu = din("b_gu", [768, 128])
    w_dn = din("w_dn", [nexp, 1536, D])
    b_dn = din("b_dn", [32, D])
    out = nc.dram_tensor("out", [2048, D], F32, kind="ExternalOutput").ap()
    dbg_out = None
    if dbg is not None:
        dbg_out = nc.dram_tensor("dbg", [32, 128, NB], F32, kind="ExternalOutput").ap()
    xres = nc.dram_tensor("xres", [32, 128, NB], F32, kind="ExternalOutput").ap()
    mixa = nc.dram_tensor("mixa", [32, 128, NB], F32, kind="ExternalOutput").ap()
    mixt = nc.dram_tensor("mixt", [32, 128, NB], BF16, kind="ExternalOutput").ap()
    x2res = nc.dram_tensor("x2res", [32, 128, NB], F32, kind="ExternalOutput").ap()
    cwtd = nc.dram_tensor("cwtd", [32, NB], F32, kind="ExternalOutput").ap()

    P = Prog()
    T_XRES, T_MIXA, T_MIXT, T_X2RES, T_CWTD, T_OUT, T_DBG = (Tile() for _ in range(7))

    ARENA_E = 106000
    with nc.sbuf_tensor("arena", [128, ARENA_E], BF16) as arena, \
            nc.psum_tensor("ps", [128, 8, 512], F32) as ps:
        off = [0]

        def alloc(shape, dt):
            n = int(np.prod(shape[1:]))
            nb = n * (2 if dt in (F32, I32) else 1)
            nb = (nb + 1) // 2 * 2
            v = arena[0:shape[0], off[0]:off[0] + nb]
            off[0] += nb
            assert off[0] <= ARENA_E, ("arena overflow", off[0])
            if dt != BF16:
                v = v.bitcast(dt)
            if len(shape) == 3:
                v = v.rearrange("p (a b) -> p a b", b=shape[2])
            return v

        A = alloc([128, 32, NB], BF16)
        TA = Tile()
        FBraw = arena[:, off[0]:off[0] + 32 * NB * 2]
        off[0] += 32 * NB * 2
        Fv = FBraw.bitcast(F32).rearrange("p (a b) -> p a b", b=NB)
        Bv = FBraw[:, 0:32 * NB].rearrange("p (a b) -> p a b", b=NB)
        TF = [Tile() for _ in range(32)]
        TB = Tile()
        W = [alloc([128, 8192], BF16) for _ in range(3)]
        TW = [Tile() for _ in range(3)]
        XH = alloc([128, 32, 128], BF16)
        TXH = Tile()
        TC = Tile()
        ident_f = alloc([128, 128], F32)
        ident_bf = alloc([128, 128], BF16)
        ones_f = alloc([128, 128], F32)
        ones_bf = alloc([128, 128], BF16)
        rotm_bf = alloc([32, 32], BF16)
        mk_bf = alloc([128, 3, 512], BF16)
        PV = alloc([128, 512], F32)
        DV = alloc([128, 3, 32], F32)
        BGU = alloc([128, 768], F32)
        WR = alloc([128, 32, 32], F32)
        BR = alloc([128, 32], F32)
        ESINK = alloc([128, 32], F32)
        INVF = alloc([32, 1], F32)
        FLAG = alloc([128, 1], F32)
        HC = alloc([128, 32], F32)
        THC = Tile()
        XLT = alloc([128, 32, 3], F32)
        TXLT = Tile()
        KMT = alloc([128, 4, 256], BF16)
        VM = alloc([128, 2, 512], BF16)
        CWT = alloc([32, NB], F32)
        TCWT = Tile()
        CS = alloc([32, 2, 640], F32)
        Trope = Tile()
        ov_base = off[0]
        TPS = [Tile() for _ in range(8)]
        for _t in TPS:
            _t.psum = True
        pb = [0]
        wr = [0]

        def bank():
            b = pb[0]
            pb[0] = (b + 1) % 8
            return b

        def wslot():
            s = wr[0]
            wr[0] = (s + 1) % 3
            return s

        def ov_reset():
            P.fence()
            off[0] = ov_base

        V_BGA, V_BGL, V_CW0, V_CB, V_BLA, V_BLI, V_LAM = 0, 1, 2, 6, 7, 8, 9
        V_L1G, V_L1B, V_L2G, V_L2B, V_L3G, V_L3B = 10, 11, 12, 13, 14, 15

        def pv(v, c):
            return PV[:, v * 32 + c: v * 32 + c + 1]

        def act(out_, in_, func, reads, writes, bias=None, scale=None):
            kw = {}
            if bias is not None:
                kw['bias'] = bias
            if scale is not None:
                kw['scale'] = scale
            P.op('act', lambda e: e.activation(out=out_, in_=in_, func=func, **kw), reads, writes)

        def tsc(out_, in0, s1, s2, op0, op1, reads, writes):
            if s2 is None:
                P.op('dve', lambda e: e.tensor_scalar(out=out_, in0=in0, scalar1=s1, scalar2=None, op0=op0), reads, writes)
            else:
                P.op('dve', lambda e: e.tensor_scalar(out=out_, in0=in0, scalar1=s1, scalar2=s2, op0=op0, op1=op1), reads, writes)

        def tt(out_, in0, in1, op, reads, writes):
            P.op('dve', lambda e: e.tensor_tensor(out=out_, in0=in0, in1=in1, op=op), reads, writes)

        def stt(out_, in0, s, in1, op0, op1, reads, writes):
            P.op('dve', lambda e: e.scalar_tensor_tensor(out=out_, in0=in0, scalar=s, in1=in1, op0=op0, op1=op1), reads, writes)

        def vcopy(out_, in_, reads, writes):
            P.op('dve', lambda e: e.tensor_copy(out=out_, in_=in_), reads, writes)

        def mmgroup(b, ps_ap, pairs, reads):
            def fn(e):
                n = len(pairs)
                ins = None
                for i, (l, r) in enumerate(pairs):
                    ins = e.matmul(ps_ap, lhsT=l, rhs=r, start=(i == 0), stop=(i == n - 1))
                return ins
            P.op('pe', fn, reads, [TPS[b]])

        def transp(b, ps_ap, in_ap, ident, reads):
            P.op('pe', lambda e: e.transpose(out=ps_ap, in_=in_ap, identity=ident), reads, [TPS[b]])

        def load_w(dram_ap):
            s = wslot()
            shp = dram_ap.shape
            n = int(np.prod(shp[1:]))
            view = W[s][:, 0:n]
            if len(shp) == 3:
                view = view.rearrange("p (a b) -> p a b", b=shp[2])
            P.dma('pool', lambda e: e.dma_start(out=view, in_=dram_ap), [], [TW[s]])
            return s, view

        def wcols(wd, r0, r1, c0, c1):
            return wd[r0:r1, c0:c1].rearrange("(kc p) n -> p kc n", p=128)

        stg = alloc([128, 4, 128], F32)
        Tstg = Tile()
        mstg = alloc([128, 3, 512], F32)
        rstg = alloc([32, 32], F32)
        bstg = alloc([128, 6, 128], F32)
        tmpv = alloc([128, 4, 32], F32)
        P.dma('sp', lambda e: e.dma_start(out=ident_f, in_=identd), [], [TC])
        P.dma('sp', lambda e: e.dma_start(out=rstg, in_=rotmd), [], [Tstg])
        P.dma('sp', lambda e: e.dma_start(out=mstg, in_=masks.rearrange("m p n -> p m n")), [], [Tstg])
        P.dma('sp', lambda e: e.dma_start(out=INVF, in_=invfd), [], [TC])
        P.dma('sp', lambda e: e.dma_start(out=FLAG, in_=flag), [], [TC])
        P.dma('sp', lambda e: e.dma_start(out=stg, in_=vecs.rearrange("(j p) x -> p j x", p=128)), [], [Tstg])
        P.dma('sp', lambda e: e.dma_start(out=bstg, in_=b_gu.rearrange("(j p) x -> p j x", p=128)), [], [Tstg])
        P.dma('sp', lambda e: e.dma_start(out=WR, in_=w_r.rearrange("(kc p) n -> p kc n", p=128)), [], [TC])
        P.dma('sp', lambda e: e.dma_start(out=BR, in_=b_r.partition_broadcast(128)), [], [TC])
        P.dma('sp', lambda e: e.dma_start(out=ESINK, in_=sinks.partition_broadcast(128)), [], [TC])
        P.op('dve', lambda e: e.memset(ones_f, 1.0 / D), [], [TC])
        P.op('dve', lambda e: e.memset(ones_bf, 1.0), [], [TC])
        P.op('dve', lambda e: e.memset(HC, 0.0), [], [THC])
        P.op('dve', lambda e: e.memset(XLT, 0.0), [], [TXLT])
        vcopy(ident_bf, ident_f, [TC], [TC])
        vcopy(rotm_bf, rstg, [Tstg], [TC])
        vcopy(mk_bf, mstg, [Tstg], [TC])
        act(ESINK, ESINK, AF.Exp, [TC], [TC])
        for j in range(4):
            b = bank()
            transp(b, ps[:, b, 0:128], stg[:, j, :], ident_f, [Tstg, TC])
            vcopy(PV[:, j * 128:(j + 1) * 128], ps[:, b, 0:128], [TPS[b]], [TC])
        for j in range(6):
            b = bank()
            transp(b, ps[:, b, 0:128], bstg[:, j, :], ident_f, [Tstg, TC])
            vcopy(BGU[:, j * 128:(j + 1) * 128], ps[:, b, 0:128], [TPS[b]], [TC])
        lam = PV[:, V_LAM * 32:(V_LAM + 1) * 32]
        t0, t1, t2, t3 = tmpv[:, 0, :], tmpv[:, 1, :], tmpv[:, 2, :], tmpv[:, 3, :]
        tsc(t0, lam, -1.0, None, ALU.mult, None, [TC], [Tstg])
        tt(t1, lam, t0, ALU.max, [TC, Tstg], [Tstg])
        act(t2, t1, AF.Exp, [Tstg], [Tstg], scale=-1.0)
        act(t2, t2, AF.Ln, [Tstg], [Tstg], bias=1.0)
        tsc(t3, t0, 0.0, None, ALU.max, None, [Tstg], [Tstg])
        tt(t3, t3, t2, ALU.add, [Tstg], [Tstg])
        tsc(DV[:, 0, :], t3, -8.0, None, ALU.mult, None, [Tstg], [TC])
        tsc(DV[:, 1, :], t3, -16.0, None, ALU.mult, None, [Tstg], [TC])

        def chk(k):
            if last is not None and k > last:
                raise _Stop()

        def _body():
            nonlocal_dummy = None
            chk(1)
            ov_reset()
            MT = alloc([128, 32, 256], BF16)
            TMT = Tile()
            xin = [alloc([128, 2048], F32)]
            Txin = [Tile()]
            for mt in range(2):
                for hf in range(2):
                    s = 0
                    P.dma('sp', lambda e, s=s, mt=mt, hf=hf, xin=xin: e.dma_start(out=xin[s], in_=memc[mt * 128:(mt + 1) * 128, hf * 2048:(hf + 1) * 2048]), [], [Txin[s]])
                    for g in range(4):
                        b = bank()
                        for j in range(4):
                            transp(b, ps[:, b, j * 128:(j + 1) * 128], xin[s][:, (g * 4 + j) * 128:(g * 4 + j + 1) * 128], ident_f, [Txin[s], TC])
                        c0 = hf * 16 + g * 4
                        act(MT[:, c0:c0 + 4, mt * 128:(mt + 1) * 128], ps[:, b, :].rearrange("p (c t) -> p c t", t=128), AF.Identity, [TPS[b]], [TMT])
            for sidx in range(2):
                s, wv = load_w(wcols(w_mkv, 0, D, sidx * 256, (sidx + 1) * 256))
                for hh in range(2):
                    h = sidx * 2 + hh
                    b = bank()
                    mmgroup(b, ps[:, b, 0:256], [(wv[:, kc, hh * 128:(hh + 1) * 128], MT[:, kc, :]) for kc in range(32)], [TW[s], TMT])
                    act(KMT[:, h, :], ps[:, b, 0:256], AF.Identity, [TPS[b]], [TC])
            for sidx in range(2):
                s, wv = load_w(wcols(w_mkv, 0, D, 512 + sidx * 256, 512 + (sidx + 1) * 256))
                for mt in range(2):
                    b = bank()
                    mmgroup(b, ps[:, b, 0:256], [(MT[:, kc, mt * 128:(mt + 1) * 128], wv[:, kc, :]) for kc in range(32)], [TW[s], TMT])
                    act(VM[:, mt, sidx * 256:(sidx + 1) * 256], ps[:, b, 0:256], AF.Identity, [TPS[b]], [TC])

            def layer_norm(vg, vb, bf_out, want_bf):
                ov_reset()
                sq = [alloc([128, NB], F32) for _ in range(2)]
                Tsq = [Tile(), Tile()]
                mean = alloc([128, NB], F32)
                rstd = alloc([128, NB], F32)
                m2 = alloc([128, NB], F32)
                tq = [alloc([128, NB], F32) for _ in range(2)]
                Ttq = [Tile(), Tile()]
                Tst = Tile()
                bm = bank()
                mmgroup(bm, ps[:, bm, :], [(ones_f, Fv[:, c, :]) for c in range(32)], [TC] + TF)
                be = bank()
                for c in range(32):
                    s = c % 2
                    act(sq[s], Fv[:, c, :], AF.Square, [TF[c]], [Tsq[s]])
                    P.op('pe', lambda e, c=c, s=s, be=be, sq=sq: e.matmul(ps[:, be, :], lhsT=ones_f, rhs=sq[s], start=(c == 0), stop=(c == 31)),
                         [TC, Tsq[s]], [TPS[be]])
                vcopy(mean, ps[:, bm, :], [TPS[bm]], [Tst])
                tt(m2, mean, mean, ALU.mult, [Tst], [Tst])
                tt(rstd, ps[:, be, :], m2, ALU.subtract, [TPS[be], Tst], [Tst])
                tsc(rstd, rstd, EPS, None, ALU.add, None, [Tst], [Tst])
                act(rstd, rstd, AF.Sqrt, [Tst], [Tst])
                P.op('dve', lambda e, rstd=rstd: e.reciprocal(out=rstd, in_=rstd), [Tst], [Tst])
                for c in range(32):
                    s = c % 2
                    tt(tq[s], Fv[:, c, :], mean, ALU.subtract, [TF[c], Tst], [Ttq[s]])
                    tt(tq[s], tq[s], rstd, ALU.mult, [Ttq[s], Tst], [Ttq[s]])
                    act(Fv[:, c, :], tq[s], AF.Identity, [Ttq[s], TC], [TF[c]], bias=pv(vb, c), scale=pv(vg, c))
                    if want_bf:
                        act(A[:, c, :], tq[s], AF.Identity, [Ttq[s], TC], [TA], bias=pv(vb, c), scale=pv(vg, c))

            def dump_dbg():
                for c in range(32):
                    P.dma('sp', lambda e, c=c: e.dma_start(out=dbg_out[c], in_=Fv[:, c, :]), [TF[c]], [T_DBG])

            for blk in blocks:
                pre = blk < 0
                r0 = 2048 + NB * blk
                chk(2)
                ov_reset()
                xin = [alloc([128, 2048], F32) for _ in range(2)]
                Txin = [Tile(), Tile()]
                xst = [alloc([128, 4, 128], F32) for _ in range(2)]
                Txst = [Tile(), Tile()]
                xi = 0
                tiles = list(range(NB // 128)) + ([] if (pre or not _DBGSW.get('halo', True)) else [-1])
                for tti in tiles:
                    for hf in range(2):
                        s = (xi % 2) if _DBGSW.get('twoslot', True) else 0
                        xi += 1
                        rr = r0 + tti * 128
                        P.dma('sp', lambda e, s=s, rr=rr, hf=hf, xin=xin: e.dma_start(out=xin[s], in_=xc[rr:rr + 128, hf * 2048:(hf + 1) * 2048]), [], [Txin[s]])
                        for g in range(4):
                            b = bank()
                            for j in range(4):
                                transp(b, ps[:, b, j * 128:(j + 1) * 128], xin[s][:, (g * 4 + j) * 128:(g * 4 + j + 1) * 128], ident_f, [Txin[s], TC])
                            c0 = hf * 16 + g * 4
                            psv = ps[:, b, :].rearrange("p (c t) -> p c t", t=128)
                            if tti >= 0:
                                act(A[:, c0:c0 + 4, tti * 128:(tti + 1) * 128], psv, AF.Identity, [TPS[b]], [TA])
                                if not pre and _DBGSW.get('copy', True):
                                    s2 = (xi * 4 + g) % 2
                                    vcopy(xst[s2].rearrange("p c t -> p (c t)"), ps[:, b, :], [TPS[b]], [Txst[s2]])
                                    if _DBGSW.get('dma', True):
                                      P.dma('sp', lambda e, s2=s2, c0=c0, tti=tti, xst=xst: e.dma_start(
                                        out=xres[c0:c0 + 4, :, tti * 128:(tti + 1) * 128].rearrange("c p t -> p c t"), in_=xst[s2]),
                                        [Txst[s2]], [T_XRES])
                            else:
                                act(XH[:, c0:c0 + 4, :], psv, AF.Identity, [TPS[b]], [TXH])

                if not pre:
                    chk(3)
                    ov_reset()
                    posi = alloc([32, 640], I32)
                    posf = alloc([32, 640], F32)
                    tfr = alloc([32, 640], F32)
                    tfi = posi
                    tff = alloc([32, 640], F32)
                    Q8 = None
                    P.dma('sp', lambda e, blk=blk, posi=posi: e.dma_start(out=posi, in_=posc[:, NB * blk:NB * blk + 640].partition_broadcast(32)), [], [Trope])
                    vcopy(posf, posi, [Trope], [Trope])
                    for which in range(2):
                        if which == 0:
                            tsc(tfr, posf, INVF[:, 0:1], 0.25, ALU.mult, ALU.add, [Trope, TC], [Trope])
                        else:
                            tsc(tfr, posf, INVF[:, 0:1], None, ALU.mult, None, [Trope, TC], [Trope])
                        vcopy(tfi, tfr, [Trope], [Trope])
                        vcopy(tff, tfi, [Trope], [Trope])
                        tt(tfr, tfr, tff, ALU.subtract, [Trope], [Trope])
                        tsc(tff, tfr, 0.5, None, ALU.is_gt, None, [Trope], [Trope])
                        tt(tfr, tfr, tff, ALU.subtract, [Trope], [Trope])
                        tsc(tff, tfr, -0.5, None, ALU.is_lt, None, [Trope], [Trope])
                        tt(tfr, tfr, tff, ALU.add, [Trope], [Trope])
                        act(CS[:, which, :], tfr, AF.Sin, [Trope], [Trope], scale=2.0 * math.pi)

                    chk(4)
                    ov_reset()
                    Q8 = alloc([128, 8, NB], BF16)
                    TQ8 = Tile()
                    KT = alloc([128, 2, 640], BF16)
                    TKT = Tile()
                    VV = alloc([128, 5, 256], BF16)
                    TVV = Tile()
                    PT = [alloc([128, 2, NB], BF16) for _ in range(2)]
                    TPT = [Tile(), Tile()]
                    rt = [alloc([32, 2, NB], F32) for _ in range(1)]
                    Trt = [Tile()]
                    dn = [alloc([128, NB], F32) for _ in range(2)]
                    Tdn = [Tile(), Tile()]
                    ri = [0]

                    def rope(b, ncols, dst, dst_tile, col0):
                        act(dst, ps[:, b, 0:ncols], AF.Identity, [TPS[b]], [dst_tile])
                        b2 = bank()
                        mmgroup(b2, ps[0:32, b2, 0:ncols], [(rotm_bf, dst[0:32, :])], [TC, dst_tile])
                        s = 0
                        tt(rt[s][:, 0, 0:ncols], ps[0:32, b, 0:ncols], CS[:, 0, col0:col0 + ncols], ALU.mult, [TPS[b], Trope], [Trt[s]])
                        tt(rt[s][:, 1, 0:ncols], ps[0:32, b2, 0:ncols], CS[:, 1, col0:col0 + ncols], ALU.mult, [TPS[b2], Trope], [Trt[s]])
                        tt(dst[0:32, :], rt[s][:, 0, 0:ncols], rt[s][:, 1, 0:ncols], ALU.add, [Trt[s]], [dst_tile])

                    for kvp in range(4):
                        for sq_ in range(4):
                            s, wv = load_w(wcols(w_in, 0, D, kvp * 1024 + sq_ * 256, kvp * 1024 + (sq_ + 1) * 256))
                            for hh in range(2):
                                b = bank()
                                mmgroup(b, ps[:, b, :], [(wv[:, kc, hh * 128:(hh + 1) * 128], A[:, kc, :]) for kc in range(32)], [TW[s], TA])
                                rope(b, NB, Q8[:, sq_ * 2 + hh, :], TQ8, 128)
                        s, wv = load_w(wcols(w_in, 0, D, 4096 + kvp * 256, 4096 + (kvp + 1) * 256))
                        for hh in range(2):
                            b = bank()
                            mmgroup(b, ps[:, b, :], [(wv[:, kc, hh * 128:(hh + 1) * 128], A[:, kc, :]) for kc in range(32)], [TW[s], TA])
                            rope(b, NB, KT[:, hh, 128:640], TKT, 128)
                            b = bank()
                            mmgroup(b, ps[:, b, 0:128], [(wv[:, kc, hh * 128:(hh + 1) * 128], XH[:, kc, :]) for kc in range(32)], [TW[s], TXH])
                            rope(b, 128, KT[:, hh, 0:128], TKT, 0)
                        s, wv = load_w(wcols(w_in, 0, D, 5120 + kvp * 256, 5120 + (kvp + 1) * 256))
                        for t5 in range(5):
                            b = bank()
                            if t5 == 0:
                                pairs = [(XH[:, kc, :], wv[:, kc, :]) for kc in range(32)]
                                rd = [TW[s], TXH]
                            else:
                                pairs = [(A[:, kc, (t5 - 1) * 128:t5 * 128], wv[:, kc, :]) for kc in range(32)]
                                rd = [TW[s], TA]
                            mmgroup(b, ps[:, b, 0:256], pairs, rd)
                            act(VV[:, t5, :], ps[:, b, 0:256], AF.Identity, [TPS[b]], [TVV])
                        for hh in range(2):
                            kv = kvp * 2 + hh
                            for n in range(4):
                                pi = (hh * 4 + n) % 2
                                for which in range(2):
                                    b = bank()
                                    kcol = (n + which) * 128
                                    if which == 1:
                                        mi = 2
                                    else:
                                        mi = 0 if (blk == 0 and n == 0) else 1
                                    pairs = [(KT[:, hh, kcol:kcol + 128], Q8[:, hh * 4:hh * 4 + 4, n * 128:(n + 1) * 128]),
                                             (ident_bf, mk_bf[:, mi, :])]
                                    mmgroup(b, ps[:, b, :], pairs, [TKT, TQ8, TC])
                                    act(PT[pi][:, which, :], ps[:, b, :], AF.Exp, [TPS[b]], [TPT[pi]], scale=SCALE)
                                bd = bank()
                                mmgroup(bd, ps[:, bd, :], [(ones_bf, PT[pi][:, 0, :]), (ones_bf, PT[pi][:, 1, :])], [TC, TPT[pi]])
                                bo = bank()
                                mmgroup(bo, ps[:, bo, :], [(VV[:, n, hh * 128:(hh + 1) * 128], PT[pi][:, 0, :]),
                                                           (VV[:, n + 1, hh * 128:(hh + 1) * 128], PT[pi][:, 1, :])], [TVV, TPT[pi]])
                                for g in range(4):
                                    h = kv * 4 + g
                                    tsc(dn[pi][:, g * 128:(g + 1) * 128], ps[:, bd, g * 128:(g + 1) * 128], ESINK[:, h:h + 1], None, ALU.add, None,
                                        [TPS[bd], TC], [Tdn[pi]])
                                P.op('dve', lambda e, pi=pi, dn=dn: e.reciprocal(out=dn[pi], in_=dn[pi]), [Tdn[pi]], [Tdn[pi]])
                                for g in range(4):
                                    tt(Bv[:, kv * 4 + g, n * 128:(n + 1) * 128], ps[:, bo, g * 128:(g + 1) * 128],
                                       dn[pi][:, g * 128:(g + 1) * 128], ALU.mult, [TPS[bo], Tdn[pi]], [TB] + TF)

                    chk(5)
                    ov_reset()
                    sg = [alloc([128, NB], F32) for _ in range(2)]
                    Tsg = [Tile(), Tile()]
                    ma = [alloc([128, NB], F32) for _ in range(2)]
                    Tma = [Tile(), Tile()]
                    for cp in range(16):
                        s1, wg = load_w(wcols(w_in, 0, D, 14336 + cp * 256, 14336 + (cp + 1) * 256))
                        s2, wp = load_w(wcols(w_ba, 0, D, cp * 256, (cp + 1) * 256))
                        for ch in range(2):
                            c = cp * 2 + ch
                            i2 = c % 2
                            bg = bank()
                            mmgroup(bg, ps[:, bg, :], [(wg[:, kc, ch * 128:(ch + 1) * 128], A[:, kc, :]) for kc in range(32)], [TW[s1], TA])
                            bp = bank()
                            mmgroup(bp, ps[:, bp, :], [(wp[:, kc, ch * 128:(ch + 1) * 128], Bv[:, kc, :]) for kc in range(32)], [TW[s2], TB])
                            act(sg[i2], ps[:, bg, :], AF.Sigmoid, [TPS[bg], TC], [Tsg[i2]], bias=pv(V_BGA, c))
                            tt(ma[i2], sg[i2], ps[:, bp, :], ALU.mult, [Tsg[i2], TPS[bp]], [Tma[i2]])
                            P.dma('sp', lambda e, c=c, i2=i2, ma=ma: e.dma_start(out=mixa[c], in_=ma[i2]), [Tma[i2]], [T_MIXA])

                chk(6)
                ov_reset()
                XL = alloc([128, 2, 516], F32)
                TXL = [Tile(), Tile()]
                XC = alloc([128, 2, NB], F32)
                TXC = [Tile(), Tile()]
                XCb = alloc([128, 2, NB], BF16)
                TXCb = Tile()
                tl = [[alloc([128, NB], F32) for _ in range(4)] for _ in range(2)]
                Ttl = [[Tile() for _ in range(4)] for _ in range(2)]
                if blk == 0:
                    tsc(HC, HC, FLAG[:, 0:1], None, ALU.mult, None, [THC, TC], [THC])
                for lb in range(16):
                    s1, wx = load_w(wcols(w_in, 0, D, 6144 + lb * 256, 6144 + (lb + 1) * 256))
                    sa_, wa = load_w(w_lru_a[lb].rearrange("(ic p) j -> p ic j", p=128))
                    si_, wi = load_w(w_lru_i[lb].rearrange("(ic p) j -> p ic j", p=128))
                    for ch in range(2):
                        c = lb * 2 + ch
                        b = bank()
                        mmgroup(b, ps[:, b, :], [(wx[:, kc, ch * 128:(ch + 1) * 128], A[:, kc, :]) for kc in range(32)], [TW[s1], TA])
                        vcopy(XL[:, ch, 0:3], XLT[:, c, :], [TXLT], [TXL[ch]])
                        vcopy(XL[:, ch, 3:515], ps[:, b, :], [TPS[b]], [TXL[ch]])
                        vcopy(XLT[:, c, :], XL[:, ch, 512:515], [TXL[ch]], [TXLT])
                        tsc(XC[:, ch, :], XL[:, ch, 0:512], pv(V_CW0 + 0, c), pv(V_CB, c), ALU.mult, ALU.add, [TXL[ch], TC], [TXC[ch]])
                        for j in range(1, 4):
                            stt(XC[:, ch, :], XL[:, ch, j:j + 512], pv(V_CW0 + j, c), XC[:, ch, :], ALU.mult, ALU.add, [TXL[ch], TC, TXC[ch]], [TXC[ch]])
                        act(XCb[:, ch, :], XC[:, ch, :], AF.Identity, [TXC[ch]], [TXCb])
                    if not pre:
                        s2, wy = load_w(wcols(w_in, 0, D, 10240 + lb * 256, 10240 + (lb + 1) * 256))
                    for ch in range(2):
                        c = lb * 2 + ch
                        R, IG, AA, A2 = tl[ch]
                        TR, TIG, TAA, TA2 = Ttl[ch]
                        U, S_, TU, TS_ = A2, AA, TA2, TAA
                        ba = bank()
                        mmgroup(ba, ps[:, ba, :], [(wa[:, ic, ch * 128:(ch + 1) * 128], XCb[:, ic, :]) for ic in range(2)], [TW[sa_], TXCb])
                        bi = bank()
                        mmgroup(bi, ps[:, bi, :], [(wi[:, ic, ch * 128:(ch + 1) * 128], XCb[:, ic, :]) for ic in range(2)], [TW[si_], TXCb])
                        act(R, ps[:, ba, :], AF.Sigmoid, [TPS[ba], TC], [TR], bias=pv(V_BLA, c))
                        act(IG, ps[:, bi, :], AF.Sigmoid, [TPS[bi], TC], [TIG], bias=pv(V_BLI, c))
                        act(AA, R, AF.Exp, [TR, TC], [TAA], scale=DV[:, 0, c:c + 1])
                        act(A2, R, AF.Exp, [TR, TC], [TA2], scale=DV[:, 1, c:c + 1])
                        tsc(A2, A2, -1.0, 1.0, ALU.mult, ALU.add, [TA2], [TA2])
                        act(A2, A2, AF.Sqrt, [TA2], [TA2])
                        tt(IG, IG, A2, ALU.mult, [TIG, TA2], [TIG])
                        tt(IG, IG, XC[:, ch, :], ALU.mult, [TIG, TXC[ch]], [TIG])
                        P.op('dve', lambda e, R=R, AA=AA, IG=IG, c=c: e.tensor_tensor_scan(out=R, data0=AA, data1=IG, initial=HC[:, c:c + 1], op0=ALU.mult, op1=ALU.add),
                             [TAA, TIG, THC], [TR])
                        vcopy(HC[:, c:c + 1], R[:, NB - 1:NB], [TR], [THC])
                        if not pre:
                            by = bank()
                            mmgroup(by, ps[:, by, :], [(wy[:, kc, ch * 128:(ch + 1) * 128], A[:, kc, :]) for kc in range(32)], [TW[s2], TA])
                            act(U, ps[:, by, :], AF.Square, [TPS[by]], [TU])
                            tsc(U, U, 0.044715, 1.0, ALU.mult, ALU.add, [TU], [TU])
                            tt(U, U, ps[:, by, :], ALU.mult, [TU, TPS[by]], [TU])
                            act(S_, U, AF.Sigmoid, [TU], [TS_], scale=1.5957691216057308)
                            tt(S_, S_, ps[:, by, :], ALU.mult, [TS_, TPS[by]], [TS_])
                            tt(Bv[:, c, :], S_, R, ALU.mult, [TS_, TR], [TB] + TF)
                if pre:
                    continue

                chk(7)
                ov_reset()
                sg = [alloc([128, NB], F32) for _ in range(2)]
                Tsg = [Tile(), Tile()]
                ma = [alloc([128, NB], F32) for _ in range(2)]
                Tma = [Tile(), Tile()]
                mo = [alloc([128, NB], BF16) for _ in range(2)]
                Tmo = [Tile(), Tile()]
                for cp in range(16):
                    s1, wg = load_w(wcols(w_in, 0, D, 18432 + cp * 256, 18432 + (cp + 1) * 256))
                    s2, wp = load_w(wcols(w_bl, 0, D, cp * 256, (cp + 1) * 256))
                    for ch in range(2):
                        c = cp * 2 + ch
                        i2 = c % 2
                        P.dma('sp', lambda e, c=c, i2=i2, ma=ma: e.dma_start(out=ma[i2], in_=mixa[c]), [T_MIXA], [Tma[i2]])
                        bg = bank()
                        mmgroup(bg, ps[:, bg, :], [(wg[:, kc, ch * 128:(ch + 1) * 128], A[:, kc, :]) for kc in range(32)], [TW[s1], TA])
                        bp = bank()
                        mmgroup(bp, ps[:, bp, :], [(wp[:, kc, ch * 128:(ch + 1) * 128], Bv[:, kc, :]) for kc in range(32)], [TW[s2], TB])
                        act(sg[i2], ps[:, bg, :], AF.Sigmoid, [TPS[bg], TC], [Tsg[i2]], bias=pv(V_BGL, c))
                        tt(sg[i2], sg[i2], ps[:, bp, :], ALU.mult, [Tsg[i2], TPS[bp]], [Tsg[i2]])
                        tt(mo[i2], sg[i2], ma[i2], ALU.add, [Tsg[i2], Tma[i2]], [Tmo[i2]])
                        P.dma('sp', lambda e, c=c, i2=i2, mo=mo: e.dma_start(out=mixt[c], in_=mo[i2]), [Tmo[i2]], [T_MIXT])

                chk(8)
                ov_reset()
                xr = [alloc([128, NB], F32) for _ in range(2)]
                Txr = [Tile(), Tile()]
                P.dma('sp', lambda e: e.dma_start(out=A, in_=mixt.rearrange("c p t -> p c t")), [T_MIXT], [TA])
                for cp in range(16):
                    s1, wv = load_w(wcols(w_mo, 0, D, cp * 256, (cp + 1) * 256))
                    for ch in range(2):
                        c = cp * 2 + ch
                        i2 = c % 2
                        P.dma('sp', lambda e, c=c, i2=i2, xr=xr: e.dma_start(out=xr[i2], in_=xres[c]), [T_XRES], [Txr[i2]])
                        b = bank()
                        mmgroup(b, ps[:, b, :], [(wv[:, kc, ch * 128:(ch + 1) * 128], A[:, kc, :]) for kc in range(32)], [TW[s1], TA])
                        stt(Fv[:, c, :], xr[i2], ALPHA, ps[:, b, :], ALU.mult, ALU.add, [Txr[i2], TPS[b]], [TF[c], TB])
                layer_norm(V_L1G, V_L1B, A, True)
                if dbg == 1:
                    dump_dbg()

                chk(9)
                ov_reset()
                QM = alloc([128, 4, NB], BF16)
                TQM = Tile()
                OM = alloc([128, 4, NB], BF16)
                TOM = Tile()
                PTm = [alloc([128, 2, NB], BF16) for _ in range(2)]
                TPTm = [Tile(), Tile()]
                dnm = [alloc([128, NB], F32) for _ in range(2)]
                Tdnm = [Tile(), Tile()]
                for sidx in range(2):
                    s, wv = load_w(wcols(w_mq, 0, D, sidx * 256, (sidx + 1) * 256))
                    for hh in range(2):
                        h = sidx * 2 + hh
                        b = bank()
                        mmgroup(b, ps[:, b, :], [(wv[:, kc, hh * 128:(hh + 1) * 128], A[:, kc, :]) for kc in range(32)], [TW[s], TA])
                        act(QM[:, h, :], ps[:, b, :], AF.Identity, [TPS[b]], [TQM])
                for h in range(4):
                    pi = h % 2
                    for mt in range(2):
                        b = bank()
                        mmgroup(b, ps[:, b, :], [(KMT[:, h, mt * 128:(mt + 1) * 128], QM[:, h, :])], [TC, TQM])
                        act(PTm[pi][:, mt, :], ps[:, b, :], AF.Exp, [TPS[b]], [TPTm[pi]], scale=SCALE)
                    bd = bank()
                    mmgroup(bd, ps[:, bd, :], [(ones_bf, PTm[pi][:, 0, :]), (ones_bf, PTm[pi][:, 1, :])], [TC, TPTm[pi]])
                    bo = bank()
                    mmgroup(bo, ps[:, bo, :], [(VM[:, 0, h * 128:(h + 1) * 128], PTm[pi][:, 0, :]), (VM[:, 1, h * 128:(h + 1) * 128], PTm[pi][:, 1, :])],
                            [TC, TPTm[pi]])
                    P.op('dve', lambda e, pi=pi, bd=bd, dnm=dnm: e.reciprocal(out=dnm[pi], in_=ps[:, bd, :]), [TPS[bd]], [Tdnm[pi]])
                    tt(OM[:, h, :], ps[:, bo, :], dnm[pi], ALU.mult, [TPS[bo], Tdnm[pi]], [TOM])
                for half in range(2):
                    s, wv = load_w(w_mout[:, half * 2048:(half + 1) * 2048].rearrange("(kc p) n -> p kc n", p=128))
                    for cc in range(16):
                        c = half * 16 + cc
                        b = bank()
                        mmgroup(b, ps[:, b, :], [(wv[:, h, cc * 128:(cc + 1) * 128], OM[:, h, :]) for h in range(4)], [TW[s], TOM])
                        stt(Fv[:, c, :], Fv[:, c, :], ALPHA, ps[:, b, :], ALU.mult, ALU.add, [TF[c], TPS[b]], [TF[c]])
                layer_norm(V_L2G, V_L2B, A, True)
                if dbg == 2:
                    dump_dbg()

                chk(10)
                ov_reset()
                L = alloc([128, 32], F32)
                M8 = alloc([128, 8], F32)
                MK = alloc([128, 32], F32)
                NM = alloc([128, 1], F32)
                SS = alloc([128, 1], F32)
                Trt_ = Tile()
                for tti in range(4):
                    b = bank()
                    mmgroup(b, ps[:, b, 0:32], [(Fv[:, c, tti * 128:(tti + 1) * 128], WR[:, c, :]) for c in range(32)], TF + [TC])
                    tt(L, ps[:, b, 0:32], BR, ALU.add, [TPS[b], TC], [Trt_])
                    P.op('dve', lambda e, M8=M8, L=L: e.max(out=M8, in_=L), [Trt_], [Trt_])
                    tsc(MK, L, M8[:, 3:4], None, ALU.is_ge, None, [Trt_], [Trt_])
                    tsc(NM, M8[:, 0:1], -1.0, None, ALU.mult, None, [Trt_], [Trt_])
                    act(L, L, AF.Exp, [Trt_], [Trt_], bias=NM[:, 0:1])
                    tt(L, L, MK, ALU.mult, [Trt_], [Trt_])
                    P.op('dve', lambda e, SS=SS, L=L: e.reduce_sum(out=SS, in_=L, axis=AX.X), [Trt_], [Trt_])
                    P.op('dve', lambda e, SS=SS: e.reciprocal(out=SS, in_=SS), [Trt_], [Trt_])
                    tsc(L, L, SS[:, 0:1], None, ALU.mult, None, [Trt_], [Trt_])
                    b2 = bank()
                    transp(b2, ps[0:32, b2, 0:128], L, ident_f, [Trt_, TC])
                    vcopy(CWT[:, tti * 128:(tti + 1) * 128], ps[0:32, b2, 0:128], [TPS[b2]], [TCWT])
                P.dma('sp', lambda e: e.dma_start(out=cwtd, in_=CWT), [TCWT], [T_CWTD])
                for c in range(32):
                    P.dma('sp', lambda e, c=c: e.dma_start(out=x2res[c], in_=Fv[:, c, :]), [TF[c]], [T_X2RES])

                chk(11)
                ov_reset()
                GLU = alloc([128, 12, NB], BF16)
                TGLU = Tile()
                CWB = [alloc([128, NB], F32) for _ in range(2)]
                TCWB = [Tile(), Tile()]
                et = [[alloc([128, NB], F32) for _ in range(3)] for _ in range(2)]
                Tet = [[Tile() for _ in range(3)] for _ in range(2)]
                for ex in range(nexp):
                    ci = ex % 2
                    P.dma('sp', lambda e, ex=ex, ci=ci, CWB=CWB: e.dma_start(out=CWB[ci], in_=cwtd[ex:ex + 1, :].partition_broadcast(128)), [T_CWTD], [TCWB[ci]])
                    for sidx in range(6):
                        s1, wg = load_w(wcols(w_gu[ex], 0, D, sidx * 256, (sidx + 1) * 256))
                        s2, wu = load_w(wcols(w_gu[ex], 0, D, 1536 + sidx * 256, 1536 + (sidx + 1) * 256))
                        for ch in range(2):
                            f = sidx * 2 + ch
                            i2 = f % 2
                            G, SG_, U1 = et[i2]
                            TG, TSG, TU1 = Tet[i2]
                            bg = bank()
                            mmgroup(bg, ps[:, bg, :], [(wg[:, kc, ch * 128:(ch + 1) * 128], A[:, kc, :]) for kc in range(32)], [TW[s1], TA])
                            bu = bank()
                            mmgroup(bu, ps[:, bu, :], [(wu[:, kc, ch * 128:(ch + 1) * 128], A[:, kc, :]) for kc in range(32)], [TW[s2], TA])
                            tsc(G, ps[:, bg, :], BGU[:, ex * 24 + f:ex * 24 + f + 1], 7.0, ALU.add, ALU.min, [TPS[bg], TC], [TG])
                            act(SG_, G, AF.Sigmoid, [TG], [TSG], scale=1.702)
                            tsc(U1, ps[:, bu, :], BGU[:, ex * 24 + 12 + f:ex * 24 + 12 + f + 1], 7.0, ALU.add, ALU.min, [TPS[bu], TC], [TU1])
                            tsc(U1, U1, -7.0, 1.0, ALU.max, ALU.add, [TU1], [TU1])
                            tt(G, G, SG_, ALU.mult, [TG, TSG], [TG])
                            tt(G, G, U1, ALU.mult, [TG, TU1], [TG])
                            tt(GLU[:, f, :], G, CWB[ci], ALU.mult, [TG, TCWB[ci]], [TGLU])
                    for sidx in range(8):
                        s1, wd = load_w(w_dn[ex][:, sidx * 512:(sidx + 1) * 512].rearrange("(kc p) n -> p kc n", p=128))
                        for ch in range(4):
                            c = sidx * 4 + ch
                            b = bank()
                            mmgroup(b, ps[:, b, :], [(wd[:, fc, ch * 128:(ch + 1) * 128], GLU[:, fc, :]) for fc in range(12)], [TW[s1], TGLU])
                            if ex == 0:
                                vcopy(Fv[:, c, :], ps[:, b, :], [TPS[b]], [TF[c]])
                            else:
                                tt(Fv[:, c, :], Fv[:, c, :], ps[:, b, :], ALU.add, [TF[c], TPS[b]], [TF[c]])

                chk(12)
                ov_reset()
                xr = [alloc([128, NB], F32) for _ in range(2)]
                Txr = [Tile(), Tile()]
                bdn = [alloc([32, 128], F32) for _ in range(2)]
                Tbdn = [Tile(), Tile()]
                for c in range(32):
                    i2 = c % 2
                    P.dma('sp', lambda e, c=c, i2=i2, xr=xr: e.dma_start(out=xr[i2], in_=x2res[c]), [T_X2RES], [Txr[i2]])
                    P.dma('sp', lambda e, c=c, i2=i2, bdn=bdn: e.dma_start(out=bdn[i2], in_=b_dn[:, c * 128:(c + 1) * 128]), [], [Tbdn[i2]])
                    b = bank()
                    mmgroup(b, ps[:, b, :], [(bdn[i2], CWT)], [Tbdn[i2], TCWT])
                    stt(Fv[:, c, :], xr[i2], ALPHA, Fv[:, c, :], ALU.mult, ALU.add, [Txr[i2], TF[c]], [TF[c]])
                    tt(Fv[:, c, :], Fv[:, c, :], ps[:, b, :], ALU.add, [TF[c], TPS[b]], [TF[c]])
                layer_norm(V_L3G, V_L3B, None, False)
                if dbg == 3:
                    dump_dbg()
                ov_reset()
                ost = [alloc([128, 2048], F32) for _ in range(2)]
                Tost = [Tile(), Tile()]
                oi = 0
                for tti in range(4):
                    for hf in range(2):
                        s = oi % 2
                        oi += 1
                        for g in range(4):
                            b = bank()
                            for j in range(4):
                                c = hf * 16 + g * 4 + j
                                transp(b, ps[:, b, j * 128:(j + 1) * 128], Fv[:, c, tti * 128:(tti + 1) * 128], ident_f, [TF[c], TC])
                            if g % 2 == 0:
                                vcopy(ost[s][:, g * 512:(g + 1) * 512], ps[:, b, :], [TPS[b]], [Tost[s]])
                            else:
                                act(ost[s][:, g * 512:(g + 1) * 512], ps[:, b, :], AF.Identity, [TPS[b]], [Tost[s]])
                        orow = NB * blk + tti * 128
                        P.dma('sp', lambda e, s=s, orow=orow, hf=hf, ost=ost: e.dma_start(out=out[orow:orow + 128, hf * 2048:(hf + 1) * 2048], in_=ost[s]),
                              [Tost[s]], [T_OUT])


        try:
            _body()
        except _Stop:
            pass
        P.fence()

        import contextlib
        with contextlib.ExitStack() as es:
            S = {}
            for e in ENGS:
                S[e] = es.enter_context(nc.semaphore("s_" + e))
            for key in P.dcnt:
                S[key] = es.enter_context(nc.semaphore("d_%s%d" % key))
            block = es.enter_context(nc.Block())

            def run(name, eng):
                for fn, waits, inck, incv in P.q[name]:
                    for k, v in waits:
                        eng.wait_ge(S[k], v)
                    if fn is not None:
                        ins = fn(eng)
                        ins.then_inc(S[inck], incv)

            @block.tensor
            def _(e):
                run('pe', e)

            @block.scalar
            def _(e):
                run('act', e)

            @block.vector
            def _(e):
                run('dve', e)

            @block.gpsimd
            def _(e):
                run('pool', e)

            @block.sync
            def _(e):
                run('sp', e)
    nc._decl = decl
    nc._P = P
    return nc


def _consts():
    ident = np.eye(128, dtype=np.float32)
    rotm = np.zeros((32, 32), np.float32)
    for m in range(16):
        rotm[m + 16, m] = -1.0
    for m in range(16, 32):
        rotm[m - 16, m] = 1.0
    invf = (1.0 / (500000.0 ** (np.arange(0, 32, 2, dtype=np.float32) / 32.0))).astype(np.float32)
    invf = (np.concatenate([invf, invf]) / np.float32(2.0 * math.pi)).astype(np.float32).reshape(32, 1)
    s = np.arange(128)[:, None]
    i = np.arange(128)[None, :]
    mprev = np.where(s > i, 0.0, NEG).astype(np.float32)
    mcur = np.where(s <= i, 0.0, NEG).astype(np.float32)
    mband = np.tile(mprev, (1, 4))
    mcur4 = np.tile(mcur, (1, 4))
    mnone = np.full((128, 512), NEG, np.float32)
    return ident, rotm, invf, mband, mcur4, mnone


def make_in_maps(inp):
    ident, rotm, invf, mband, mcur4, mnone = _consts()
    x = np.asarray(inp['x'])
    mem = np.asarray(inp['mem'])
    pos = np.asarray(inp['positions'])
    vec_list = [inp['b_gate'][0, 0], inp['b_gate'][0, 1], inp['conv_w'][0, 0], inp['conv_w'][0, 1], inp['conv_w'][0, 2],
                inp['conv_w'][0, 3], inp['conv_b'][0], inp['b_lru_a'][0].reshape(-1), inp['b_lru_i'][0].reshape(-1),
                inp['lru_lambda'][0], inp['ln1_g'][0], inp['ln1_b'][0], inp['ln2_g'][0], inp['ln2_b'][0], inp['ln3_g'][0], inp['ln3_b'][0]]
    vecs = np.ascontiguousarray(np.stack([np.asarray(v, np.float32).reshape(32, 128) for v in vec_list]).reshape(512, 128))
    shared = {
        "ident": ident, "rotm": rotm, "invf": invf,
        "w_in": np.ascontiguousarray(inp['w_in'][0]), "vecs": vecs,
        "sinks": np.ascontiguousarray(inp['attn_sinks'][0].reshape(1, 32)),
        "w_lru_a": np.ascontiguousarray(inp['w_lru_a'][0]), "w_lru_i": np.ascontiguousarray(inp['w_lru_i'][0]),
        "w_ba": np.ascontiguousarray(inp['w_branch_attn'][0]), "w_bl": np.ascontiguousarray(inp['w_branch_lru'][0]),
        "w_mo": np.ascontiguousarray(inp['w_mix_out'][0]), "w_mq": np.ascontiguousarray(inp['w_mem_q'][0]),
        "w_mkv": np.ascontiguousarray(inp['w_mem_kv'][0]), "w_mout": np.ascontiguousarray(inp['w_mem_o'][0]),
        "w_r": np.ascontiguousarray(inp['w_router'][0]), "b_r": np.ascontiguousarray(inp['b_router'][0].reshape(1, 32)),
        "w_gu": np.ascontiguousarray(inp['w_gate_up'][0]), "b_gu": np.ascontiguousarray(inp['b_gate_up'][0].reshape(768, 128)),
        "w_dn": np.ascontiguousarray(inp['w_down'][0]), "b_dn": np.ascontiguousarray(inp['b_down'][0]),
    }
    maps = []
    for core in range(8):
        b, half = core // 2, core % 2
        m = dict(shared)
        if half == 0:
            xcore = np.concatenate([np.zeros((2048, D), np.float32), x[b, 0:2048]], axis=0)
            pc = np.concatenate([np.zeros((128,), np.int32), pos[b, 0:2048].astype(np.int32)])
            mfirst = mnone
            fl = np.zeros((128, 1), np.float32)
        else:
            xcore = np.ascontiguousarray(x[b])
            pc = np.ascontiguousarray(pos[b, 1920:4096].astype(np.int32))
            mfirst = mband
            fl = np.ones((128, 1), np.float32)
        m["xc"] = np.ascontiguousarray(xcore)
        m["memc"] = np.ascontiguousarray(mem[b])
        m["posc"] = pc.reshape(1, 2176)
        m["flag"] = fl
        m["masks"] = np.ascontiguousarray(np.stack([mfirst, mband, mcur4]))
        maps.append(m)
    return maps


_NC_CACHE = {}


def kernel(**inputs):
    inp = {k: np.asarray(v) for k, v in inputs.items()}
    if "nc" not in _NC_CACHE:
        _NC_CACHE["nc"] = build_program()
    nc = _NC_CACHE["nc"]
    maps = make_in_maps(inp)
    res = run_bass_kernel_spmd(nc, maps, core_ids=list(range(8)))
    outp = np.empty((4, 4096, D), np.float32)
    for core in range(8):
        b, half = core // 2, core % 2
        outp[b, half * 2048:(half + 1) * 2048] = res.results[core]["out"]
    return outp
```

```python
import math
import numpy as np
import concourse.bass as bass
import concourse.mybir as mybir
from concourse.bass_utils import run_bass_kernel_spmd

F32 = mybir.dt.float32
BF16 = mybir.dt.bfloat16
I32 = mybir.dt.int32
AF = mybir.ActivationFunctionType
ALU = mybir.AluOpType
AX = mybir.AxisListType

D = 4096
NB = 512
ALPHA = 2.0 ** 0.25
EPS = 1e-5
SCALE = 128.0 ** -0.5
NEG = -30000.0
ENGS = ['pe', 'act', 'dve', 'pool', 'sp']
NDS = 8


class Tile:
    __slots__ = ("w", "r", "psum")

    def __init__(self):
        self.w = {}
        self.r = {}
        self.psum = False


class Prog:
    def __init__(self):
        self.q = {e: [] for e in ENGS}
        self.ms = {e: 0 for e in ENGS}
        self.waited = {}
        self.dcnt = {(qe, j): 0 for qe in ('sp', 'pool') for j in range(NDS)}
        self.drr = {'sp': 0, 'pool': 0}

    def _collect(self, eng, reads, writes):
        need = {}
        for t in reads:
            for k, v in t.w.items():
                if need.get(k, 0) < v:
                    need[k] = v
        for t in writes:
            for k, v in t.w.items():
                if need.get(k, 0) < v:
                    need[k] = v
            for k, v in t.r.items():
                if need.get(k, 0) < v:
                    need[k] = v
        waits = []
        for k, v in need.items():
            if k == eng and eng == 'pe':
                continue
            if self.waited.get((eng, k), 0) < v:
                self.waited[(eng, k)] = v
                waits.append((k, v))
        return waits

    def _mark(self, tok, reads, writes):
        k, v = tok
        for t in reads:
            if t.r.get(k, 0) < v:
                t.r[k] = v
        for t in writes:
            if t.w.get(k, 0) < v:
                t.w[k] = v

    def op(self, eng, fn, reads=(), writes=()):
        if eng != 'pe':
            ex = [t for t in reads if getattr(t, 'psum', False)]
            if ex:
                writes = list(writes) + ex
        waits = self._collect(eng, reads, writes)
        self.ms[eng] += 1
        tok = (eng, self.ms[eng])
        self.q[eng].append((fn, waits, eng, 1))
        self._mark(tok, reads, writes)

    def dma(self, qe, fn, reads=(), writes=()):
        j = self.drr[qe]
        self.drr[qe] = (j + 1) % NDS
        key = (qe, j)
        waits = self._collect(qe, reads, writes)
        prev = 16 * self.dcnt[key]
        if prev > 0 and self.waited.get((qe, key), 0) < prev:
            self.waited[(qe, key)] = prev
            waits.append((key, prev))
        self.dcnt[key] += 1
        tok = (key, 16 * self.dcnt[key])
        self.q[qe].append((fn, waits, key, 16))
        self._mark(tok, reads, writes)

    def fence(self):
        for e in ENGS:
            waits = []
            for k in ENGS:
                if k == e:
                    continue
                v = self.ms[k]
                if v > 0 and self.waited.get((e, k), 0) < v:
                    self.waited[(e, k)] = v
                    waits.append((k, v))
            for key, c in self.dcnt.items():
                v = 16 * c
                if v > 0 and self.waited.get((e, key), 0) < v:
                    self.waited[(e, key)] = v
                    waits.append((key, v))
            if waits:
                self.q[e].append((None, waits, None, 0))


_DBGSW = {}


class _Stop(Exception):
    pass


def build_program(blocks=(-4, -3, -2, -1, 0, 1, 2, 3), nexp=32, dbg=None, last=None):
    nc = bass.Bass("TRN2", target_bir_lowering=False)

    decl = {}
    need_lvl = {"w_in": 3, "w_ba": 5, "w_bl": 7, "w_mo": 8, "w_mq": 9, "w_mout": 9, "w_gu": 11, "w_dn": 11,
                "w_lru_a": 6, "w_lru_i": 6, "w_mkv": 1}

    def din(name, shape, dt=F32):
        if last is not None and name in need_lvl and need_lvl[name] > last:
            shape = [128, 128]
        decl[name] = list(shape)
        return nc.dram_tensor(name, shape, dt, kind="ExternalInput").ap()

    xc = din("xc", [4096, D])
    memc = din("memc", [256, D])
    posc = din("posc", [1, 2176], I32)
    flag = din("flag", [128, 1])
    masks = din("masks", [3, 128, 512])
    identd = din("ident", [128, 128])
    rotmd = din("rotm", [32, 32])
    invfd = din("invf", [32, 1])
    w_in = din("w_in", [D, 22528])
    vecs = din("vecs", [512, 128])
    sinks = din("sinks", [1, 32])
    w_lru_a = din("w_lru_a", [16, 256, 256])
    w_lru_i = din("w_lru_i", [16, 256, 256])
    w_ba = din("w_ba", [D, D])
    w_bl = din("w_bl", [D, D])
    w_mo = din("w_mo", [D, D])
    w_mq = din("w_mq", [D, 512])
    w_mkv = din("w_mkv", [D, 1024])
    w_mout = din("w_mout", [512, D])
    w_r = din("w_r", [D, 32])
    b_r = din("b_r", [1, 32])
    w_gu = din("w_gu", [nexp, D, 3072])
    b_gu = din("b_gu", [768, 128])
    w_dn = din("w_dn", [nexp, 1536, D])
    b_dn = din("b_dn", [32, D])
    out = nc.dram_tensor("out", [2048, D], F32, kind="ExternalOutput").ap()
    dbg_out = None
    if dbg is not None:
        dbg_out = nc.dram_tensor("dbg", [32, 128, NB], F32, kind="ExternalOutput").ap()
    xres = nc.dram_tensor("xres", [32, 128, NB], F32, kind="ExternalOutput").ap()
    mixa = nc.dram_tensor("mixa", [32, 128, NB], F32, kind="ExternalOutput").ap()
    mixt = nc.dram_tensor("mixt", [32, 128, NB], BF16, kind="ExternalOutput").ap()
    x2res = nc.dram_tensor("x2res", [32, 128, NB], F32, kind="ExternalOutput").ap()
    cwtd = nc.dram_tensor("cwtd", [32, NB], F32, kind="ExternalOutput").ap()

    P = Prog()
    T_XRES, T_MIXA, T_MIXT, T_X2RES, T_CWTD, T_OUT, T_DBG = (Tile() for _ in range(7))

    ARENA_E = 106000
    with nc.sbuf_tensor("arena", [128, ARENA_E], BF16) as arena, \
            nc.psum_tensor("ps", [128, 8, 512], F32) as ps:
        off = [0]

        def alloc(shape, dt):
            n = int(np.prod(shape[1:]))
            nb = n * (2 if dt in (F32, I32) else 1)
            nb = (nb + 1) // 2 * 2
            v = arena[0:shape[0], off[0]:off[0] + nb]
            off[0] += nb
            assert off[0] <= ARENA_E, ("arena overflow", off[0])
            if dt != BF16:
                v = v.bitcast(dt)
            if len(shape) == 3:
                v = v.rearrange("p (a b) -> p a b", b=shape[2])
            return v

        A = alloc([128, 32, NB], BF16)
        TA = Tile()
        FBraw = arena[:, off[0]:off[0] + 32 * NB * 2]
        off[0] += 32 * NB * 2
        Fv = FBraw.bitcast(F32).rearrange("p (a b) -> p a b", b=NB)
        Bv = FBraw[:, 0:32 * NB].rearrange("p (a b) -> p a b", b=NB)
        TF = [Tile() for _ in range(32)]
        TB = Tile()
        W = [alloc([128, 8192], BF16) for _ in range(3)]
        TW = [Tile() for _ in range(3)]
        XH = alloc([128, 32, 128], BF16)
        TXH = Tile()
        TC = Tile()
        ident_f = alloc([128, 128], F32)
        ident_bf = alloc([128, 128], BF16)
        ones_f = alloc([128, 128], F32)
        ones_bf = alloc([128, 128], BF16)
        rotm_bf = alloc([32, 32], BF16)
        mk_bf = alloc([128, 3, 512], BF16)
        PV = alloc([128, 512], F32)
        DV = alloc([128, 3, 32], F32)
        BGU = alloc([128, 768], F32)
        WR = alloc([128, 32, 32], F32)
        BR = alloc([128, 32], F32)
        ESINK = alloc([128, 32], F32)
        INVF = alloc([32, 1], F32)
        FLAG = alloc([128, 1], F32)
        HC = alloc([128, 32], F32)
        THC = Tile()
        XLT = alloc([128, 32, 3], F32)
        TXLT = Tile()
        KMT = alloc([128, 4, 256], BF16)
        VM = alloc([128, 2, 512], BF16)
        CWT = alloc([32, NB], F32)
        TCWT = Tile()
        CS = alloc([32, 2, 640], F32)
        Trope = Tile()
        ov_base = off[0]
        TPS = [Tile() for _ in range(8)]
        for _t in TPS:
            _t.psum = True
        pb = [0]
        wr = [0]

        def bank():
            b = pb[0]
            pb[0] = (b + 1) % 8
            return b

        def wslot():
            s = wr[0]
            wr[0] = (s + 1) % 3
            return s

        def ov_reset():
            P.fence()
            off[0] = ov_base

        V_BGA, V_BGL, V_CW0, V_CB, V_BLA, V_BLI, V_LAM = 0, 1, 2, 6, 7, 8, 9
        V_L1G, V_L1B, V_L2G, V_L2B, V_L3G, V_L3B = 10, 11, 12, 13, 14, 15

        def pv(v, c):
            return PV[:, v * 32 + c: v * 32 + c + 1]

        def act(out_, in_, func, reads, writes, bias=None, scale=None):
            kw = {}
            if bias is not None:
                kw['bias'] = bias
            if scale is not None:
                kw['scale'] = scale
            P.op('act', lambda e: e.activation(out=out_, in_=in_, func=func, **kw), reads, writes)

        def tsc(out_, in0, s1, s2, op0, op1, reads, writes):
            if s2 is None:
                P.op('dve', lambda e: e.tensor_scalar(out=out_, in0=in0, scalar1=s1, scalar2=None, op0=op0), reads, writes)
            else:
                P.op('dve', lambda e: e.tensor_scalar(out=out_, in0=in0, scalar1=s1, scalar2=s2, op0=op0, op1=op1), reads, writes)

        def tt(out_, in0, in1, op, reads, writes):
            P.op('dve', lambda e: e.tensor_tensor(out=out_, in0=in0, in1=in1, op=op), reads, writes)

        def stt(out_, in0, s, in1, op0, op1, reads, writes):
            P.op('dve', lambda e: e.scalar_tensor_tensor(out=out_, in0=in0, scalar=s, in1=in1, op0=op0, op1=op1), reads, writes)

        def vcopy(out_, in_, reads, writes):
            P.op('dve', lambda e: e.tensor_copy(out=out_, in_=in_), reads, writes)

        def mmgroup(b, ps_ap, pairs, reads):
            def fn(e):
                n = len(pairs)
                ins = None
                for i, (l, r) in enumerate(pairs):
                    ins = e.matmul(ps_ap, lhsT=l, rhs=r, start=(i == 0), stop=(i == n - 1))
                return ins
            P.op('pe', fn, reads, [TPS[b]])

        def transp(b, ps_ap, in_ap, ident, reads):
            P.op('pe', lambda e: e.transpose(out=ps_ap, in_=in_ap, identity=ident), reads, [TPS[b]])

        def load_w(dram_ap):
            s = wslot()
            shp = dram_ap.shape
            n = int(np.prod(shp[1:]))
            view = W[s][:, 0:n]
            if len(shp) == 3:
                view = view.rearrange("p (a b) -> p a b", b=shp[2])
            P.dma('pool', lambda e: e.dma_start(out=view, in_=dram_ap), [], [TW[s]])
            return s, view

        def wcols(wd, r0, r1, c0, c1):
            return wd[r0:r1, c0:c1].rearrange("(kc p) n -> p kc n", p=128)

        stg = alloc([128, 4, 128], F32)
        Tstg = Tile()
        mstg = alloc([128, 3, 512], F32)
        rstg = alloc([32, 32], F32)
        bstg = alloc([128, 6, 128], F32)
        tmpv = alloc([128, 4, 32], F32)
        P.dma('sp', lambda e: e.dma_start(out=ident_f, in_=identd), [], [TC])
        P.dma('sp', lambda e: e.dma_start(out=rstg, in_=rotmd), [], [Tstg])
        P.dma('sp', lambda e: e.dma_start(out=mstg, in_=masks.rearrange("m p n -> p m n")), [], [Tstg])
        P.dma('sp', lambda e: e.dma_start(out=INVF, in_=invfd), [], [TC])
        P.dma('sp', lambda e: e.dma_start(out=FLAG, in_=flag), [], [TC])
        P.dma('sp', lambda e: e.dma_start(out=stg, in_=vecs.rearrange("(j p) x -> p j x", p=128)), [], [Tstg])
        P.dma('sp', lambda e: e.dma_start(out=bstg, in_=b_gu.rearrange("(j p) x -> p j x", p=128)), [], [Tstg])
        P.dma('sp', lambda e: e.dma_start(out=WR, in_=w_r.rearrange("(kc p) n -> p kc n", p=128)), [], [TC])
        P.dma('sp', lambda e: e.dma_start(out=BR, in_=b_r.partition_broadcast(128)), [], [TC])
        P.dma('sp', lambda e: e.dma_start(out=ESINK, in_=sinks.partition_broadcast(128)), [], [TC])
        P.op('dve', lambda e: e.memset(ones_f, 1.0 / D), [], [TC])
        P.op('dve', lambda e: e.memset(ones_bf, 1.0), [], [TC])
        P.op('dve', lambda e: e.memset(HC, 0.0), [], [THC])
        P.op('dve', lambda e: e.memset(XLT, 0.0), [], [TXLT])
        vcopy(ident_bf, ident_f, [TC], [TC])
        vcopy(rotm_bf, rstg, [Tstg], [TC])
        vcopy(mk_bf, mstg, [Tstg], [TC])
        act(ESINK, ESINK, AF.Exp, [TC], [TC])
        for j in range(4):
            b = bank()
            transp(b, ps[:, b, 0:128], stg[:, j, :], ident_f, [Tstg, TC])
            vcopy(PV[:, j * 128:(j + 1) * 128], ps[:, b, 0:128], [TPS[b]], [TC])
        for j in range(6):
            b = bank()
            transp(b, ps[:, b, 0:128], bstg[:, j, :], ident_f, [Tstg, TC])
            vcopy(BGU[:, j * 128:(j + 1) * 128], ps[:, b, 0:128], [TPS[b]], [TC])
        lam = PV[:, V_LAM * 32:(V_LAM + 1) * 32]
        t0, t1, t2, t3 = tmpv[:, 0, :], tmpv[:, 1, :], tmpv[:, 2, :], tmpv[:, 3, :]
        tsc(t0, lam, -1.0, None, ALU.mult, None, [TC], [Tstg])
        tt(t1, lam, t0, ALU.max, [TC, Tstg], [Tstg])
        act(t2, t1, AF.Exp, [Tstg], [Tstg], scale=-1.0)
        act(t2, t2, AF.Ln, [Tstg], [Tstg], bias=1.0)
        tsc(t3, t0, 0.0, None, ALU.max, None, [Tstg], [Tstg])
        tt(t3, t3, t2, ALU.add, [Tstg], [Tstg])
        tsc(DV[:, 0, :], t3, -8.0, None, ALU.mult, None, [Tstg], [TC])
        tsc(DV[:, 1, :], t3, -16.0, None, ALU.mult, None, [Tstg], [TC])

        def chk(k):
            if last is not None and k > last:
                raise _Stop()

        def _body():
            nonlocal_dummy = None
            chk(1)
            ov_reset()
            MT = alloc([128, 32, 256], BF16)
            TMT = Tile()
            xin = [alloc([128, 2048], F32)]
            Txin = [Tile()]
            for mt in range(2):
                for hf in range(2):
                    s = 0
                    P.dma('sp', lambda e, s=s, mt=mt, hf=hf, xin=xin: e.dma_start(out=xin[s], in_=memc[mt * 128:(mt + 1) * 128, hf * 2048:(hf + 1) * 2048]), [], [Txin[s]])
                    for g in range(4):
                        b = bank()
                        for j in range(4):
                            transp(b, ps[:, b, j * 128:(j + 1) * 128], xin[s][:, (g * 4 + j) * 128:(g * 4 + j + 1) * 128], ident_f, [Txin[s], TC])
                        c0 = hf * 16 + g * 4
                        act(MT[:, c0:c0 + 4, mt * 128:(mt + 1) * 128], ps[:, b, :].rearrange("p (c t) -> p c t", t=128), AF.Identity, [TPS[b]], [TMT])
            for sidx in range(2):
                s, wv = load_w(wcols(w_mkv, 0, D, sidx * 256, (sidx + 1) * 256))
                for hh in range(2):
                    h = sidx * 2 + hh
                    b = bank()
                    mmgroup(b, ps[:, b, 0:256], [(wv[:, kc, hh * 128:(hh + 1) * 128], MT[:, kc, :]) for kc in range(32)], [TW[s], TMT])
                    act(KMT[:, h, :], ps[:, b, 0:256], AF.Identity, [TPS[b]], [TC])
            for sidx in range(2):
                s, wv = load_w(wcols(w_mkv, 0, D, 512 + sidx * 256, 512 + (sidx + 1) * 256))
                for mt in range(2):
                    b = bank()
                    mmgroup(b, ps[:, b, 0:256], [(MT[:, kc, mt * 128:(mt + 1) * 128], wv[:, kc, :]) for kc in range(32)], [TW[s], TMT])
                    act(VM[:, mt, sidx * 256:(sidx + 1) * 256], ps[:, b, 0:256], AF.Identity, [TPS[b]], [TC])

            def layer_norm(vg, vb, bf_out, want_bf):
                ov_reset()
                sq = [alloc([128, NB], F32) for _ in range(2)]
                Tsq = [Tile(), Tile()]
                mean = alloc([128, NB], F32)
                rstd = alloc([128, NB], F32)
                m2 = alloc([128, NB], F32)
                tq = [alloc([128, NB], F32) for _ in range(2)]
                Ttq = [Tile(), Tile()]
                Tst = Tile()
                bm = bank()
                mmgroup(bm, ps[:, bm, :], [(ones_f, Fv[:, c, :]) for c in range(32)], [TC] + TF)
                be = bank()
                for c in range(32):
                    s = c % 2
                    act(sq[s], Fv[:, c, :], AF.Square, [TF[c]], [Tsq[s]])
                    P.op('pe', lambda e, c=c, s=s, be=be, sq=sq: e.matmul(ps[:, be, :], lhsT=ones_f, rhs=sq[s], start=(c == 0), stop=(c == 31)),
                         [TC, Tsq[s]], [TPS[be]])
                vcopy(mean, ps[:, bm, :], [TPS[bm]], [Tst])
                tt(m2, mean, mean, ALU.mult, [Tst], [Tst])
                tt(rstd, ps[:, be, :], m2, ALU.subtract, [TPS[be], Tst], [Tst])
                tsc(rstd, rstd, EPS, None, ALU.add, None, [Tst], [Tst])
                act(rstd, rstd, AF.Sqrt, [Tst], [Tst])
                P.op('dve', lambda e, rstd=rstd: e.reciprocal(out=rstd, in_=rstd), [Tst], [Tst])
                for c in range(32):
                    s = c % 2
                    tt(tq[s], Fv[:, c, :], mean, ALU.subtract, [TF[c], Tst], [Ttq[s]])
                    tt(tq[s], tq[s], rstd, ALU.mult, [Ttq[s], Tst], [Ttq[s]])
                    act(Fv[:, c, :], tq[s], AF.Identity, [Ttq[s], TC], [TF[c]], bias=pv(vb, c), scale=pv(vg, c))
                    if want_bf:
                        act(A[:, c, :], tq[s], AF.Identity, [Ttq[s], TC], [TA], bias=pv(vb, c), scale=pv(vg, c))

            def dump_dbg():
                for c in range(32):
                    P.dma('sp', lambda e, c=c: e.dma_start(out=dbg_out[c], in_=Fv[:, c, :]), [TF[c]], [T_DBG])

            for blk in blocks:
                pre = blk < 0
                r0 = 2048 + NB * blk
                chk(2)
                ov_reset()
                xin = [alloc([128, 2048], F32) for _ in range(2)]
                Txin = [Tile(), Tile()]
                xst = [alloc([128, 4, 128], F32) for _ in range(2)]
                Txst = [Tile(), Tile()]
                xi = 0
                tiles = list(range(NB // 128)) + ([] if (pre or not _DBGSW.get('halo', True)) else [-1])
                for tti in tiles:
                    for hf in range(2):
                        s = (xi % 2) if _DBGSW.get('twoslot', False) else 0
                        xi += 1
                        rr = r0 + tti * 128
                        P.dma('sp', lambda e, s=s, rr=rr, hf=hf, xin=xin: e.dma_start(out=xin[s], in_=xc[rr:rr + 128, hf * 2048:(hf + 1) * 2048]), [], [Txin[s]])
                        for g in range(4):
                            b = bank()
                            for j in range(4):
                                transp(b, ps[:, b, j * 128:(j + 1) * 128], xin[s][:, (g * 4 + j) * 128:(g * 4 + j + 1) * 128], ident_f, [Txin[s], TC])
                            c0 = hf * 16 + g * 4
                            psv = ps[:, b, :].rearrange("p (c t) -> p c t", t=128)
                            if tti >= 0:
                                act(A[:, c0:c0 + 4, tti * 128:(tti + 1) * 128], psv, AF.Identity, [TPS[b]], [TA])
                                if not pre and _DBGSW.get('copy', True):
                                    s2 = (xi * 4 + g) % 2
                                    vcopy(xst[s2].rearrange("p c t -> p (c t)"), ps[:, b, :], [TPS[b]], [Txst[s2]])
                                    if _DBGSW.get('dma', True):
                                      P.dma('sp', lambda e, s2=s2, c0=c0, tti=tti, xst=xst: e.dma_start(
                                        out=xres[c0:c0 + 4, :, tti * 128:(tti + 1) * 128].rearrange("c p t -> p c t"), in_=xst[s2]),
                                        [Txst[s2]], [T_XRES])
                            else:
                                act(XH[:, c0:c0 + 4, :], psv, AF.Identity, [TPS[b]], [TXH])

                if not pre:
                    chk(3)
                    ov_reset()
                    posi = alloc([32, 640], I32)
                    posf = alloc([32, 640], F32)
                    tfr = alloc([32, 640], F32)
                    tfi = posi
                    tff = alloc([32, 640], F32)
                    Q8 = None
                    P.dma('sp', lambda e, blk=blk, posi=posi: e.dma_start(out=posi, in_=posc[:, NB * blk:NB * blk + 640].partition_broadcast(32)), [], [Trope])
                    vcopy(posf, posi, [Trope], [Trope])
                    for which in range(2):
                        if which == 0:
                            tsc(tfr, posf, INVF[:, 0:1], 0.25, ALU.mult, ALU.add, [Trope, TC], [Trope])
                        else:
                            tsc(tfr, posf, INVF[:, 0:1], None, ALU.mult, None, [Trope, TC], [Trope])
                        vcopy(tfi, tfr, [Trope], [Trope])
                        vcopy(tff, tfi, [Trope], [Trope])
                        tt(tfr, tfr, tff, ALU.subtract, [Trope], [Trope])
                        tsc(tff, tfr, 0.5, None, ALU.is_gt, None, [Trope], [Trope])
                        tt(tfr, tfr, tff, ALU.subtract, [Trope], [Trope])
                        tsc(tff, tfr, -0.5, None, ALU.is_lt, None, [Trope], [Trope])
                        tt(tfr, tfr, tff, ALU.add, [Trope], [Trope])
                        act(CS[:, which, :], tfr, AF.Sin, [Trope], [Trope], scale=2.0 * math.pi)

                    chk(4)
                    ov_reset()
                    Q8 = alloc([128, 8, NB], BF16)
                    TQ8 = Tile()
                    KT = alloc([128, 2, 640], BF16)
                    TKT = Tile()
                    VV = alloc([128, 5, 256], BF16)
                    TVV = Tile()
                    PT = [alloc([128, 2, NB], BF16) for _ in range(2)]
                    TPT = [Tile(), Tile()]
                    rt = [alloc([32, 2, NB], F32) for _ in range(1)]
                    Trt = [Tile()]
                    dn = [alloc([128, NB], F32) for _ in range(2)]
                    Tdn = [Tile(), Tile()]
                    ri = [0]

                    def rope(b, ncols, dst, dst_tile, col0):
                        act(dst, ps[:, b, 0:ncols], AF.Identity, [TPS[b]], [dst_tile])
                        b2 = bank()
                        mmgroup(b2, ps[0:32, b2, 0:ncols], [(rotm_bf, dst[0:32, :])], [TC, dst_tile])
                        s = 0
                        tt(rt[s][:, 0, 0:ncols], ps[0:32, b, 0:ncols], CS[:, 0, col0:col0 + ncols], ALU.mult, [TPS[b], Trope], [Trt[s]])
                        tt(rt[s][:, 1, 0:ncols], ps[0:32, b2, 0:ncols], CS[:, 1, col0:col0 + ncols], ALU.mult, [TPS[b2], Trope], [Trt[s]])
                        tt(dst[0:32, :], rt[s][:, 0, 0:ncols], rt[s][:, 1, 0:ncols], ALU.add, [Trt[s]], [dst_tile])

                    for kvp in range(4):
                        for sq_ in range(4):
                            s, wv = load_w(wcols(w_in, 0, D, kvp * 1024 + sq_ * 256, kvp * 1024 + (sq_ + 1) * 256))
                            for hh in range(2):
                                b = bank()
                                mmgroup(b, ps[:, b, :], [(wv[:, kc, hh * 128:(hh + 1) * 128], A[:, kc, :]) for kc in range(32)], [TW[s], TA])
                                rope(b, NB, Q8[:, sq_ * 2 + hh, :], TQ8, 128)
                        s, wv = load_w(wcols(w_in, 0, D, 4096 + kvp * 256, 4096 + (kvp + 1) * 256))
                        for hh in range(2):
                            b = bank()
                            mmgroup(b, ps[:, b, :], [(wv[:, kc, hh * 128:(hh + 1) * 128], A[:, kc, :]) for kc in range(32)], [TW[s], TA])
                            rope(b, NB, KT[:, hh, 128:640], TKT, 128)
                            b = bank()
                            mmgroup(b, ps[:, b, 0:128], [(wv[:, kc, hh * 128:(hh + 1) * 128], XH[:, kc, :]) for kc in range(32)], [TW[s], TXH])
                            rope(b, 128, KT[:, hh, 0:128], TKT, 0)
                        s, wv = load_w(wcols(w_in, 0, D, 5120 + kvp * 256, 5120 + (kvp + 1) * 256))
                        for t5 in range(5):
                            b = bank()
                            if t5 == 0:
                                pairs = [(XH[:, kc, :], wv[:, kc, :]) for kc in range(32)]
                                rd = [TW[s], TXH]
                            else:
                                pairs = [(A[:, kc, (t5 - 1) * 128:t5 * 128], wv[:, kc, :]) for kc in range(32)]
                                rd = [TW[s], TA]
                            mmgroup(b, ps[:, b, 0:256], pairs, rd)
                            act(VV[:, t5, :], ps[:, b, 0:256], AF.Identity, [TPS[b]], [TVV])
                        for hh in range(2):
                            kv = kvp * 2 + hh
                            for n in range(4):
                                pi = (hh * 4 + n) % 2
                                for which in range(2):
                                    b = bank()
                                    kcol = (n + which) * 128
                                    if which == 1:
                                        mi = 2
                                    else:
                                        mi = 0 if (blk == 0 and n == 0) else 1
                                    pairs = [(KT[:, hh, kcol:kcol + 128], Q8[:, hh * 4:hh * 4 + 4, n * 128:(n + 1) * 128]),
                                             (ident_bf, mk_bf[:, mi, :])]
                                    mmgroup(b, ps[:, b, :], pairs, [TKT, TQ8, TC])
                                    act(PT[pi][:, which, :], ps[:, b, :], AF.Exp, [TPS[b]], [TPT[pi]], scale=SCALE)
                                bd = bank()
                                mmgroup(bd, ps[:, bd, :], [(ones_bf, PT[pi][:, 0, :]), (ones_bf, PT[pi][:, 1, :])], [TC, TPT[pi]])
                                bo = bank()
                                mmgroup(bo, ps[:, bo, :], [(VV[:, n, hh * 128:(hh + 1) * 128], PT[pi][:, 0, :]),
                                                           (VV[:, n + 1, hh * 128:(hh + 1) * 128], PT[pi][:, 1, :])], [TVV, TPT[pi]])
                                for g in range(4):
                                    h = kv * 4 + g
                                    tsc(dn[pi][:, g * 128:(g + 1) * 128], ps[:, bd, g * 128:(g + 1) * 128], ESINK[:, h:h + 1], None, ALU.add, None,
                                        [TPS[bd], TC], [Tdn[pi]])
                                P.op('dve', lambda e, pi=pi, dn=dn: e.reciprocal(out=dn[pi], in_=dn[pi]), [Tdn[pi]], [Tdn[pi]])
                                for g in range(4):
                                    tt(Bv[:, kv * 4 + g, n * 128:(n + 1) * 128], ps[:, bo, g * 128:(g + 1) * 128],
                                       dn[pi][:, g * 128:(g + 1) * 128], ALU.mult, [TPS[bo], Tdn[pi]], [TB] + TF)

                    chk(5)
                    ov_reset()
                    sg = [alloc([128, NB], F32) for _ in range(2)]
                    Tsg = [Tile(), Tile()]
                    ma = [alloc([128, NB], F32) for _ in range(2)]
                    Tma = [Tile(), Tile()]
                    for cp in range(16):
                        s1, wg = load_w(wcols(w_in, 0, D, 14336 + cp * 256, 14336 + (cp + 1) * 256))
                        s2, wp = load_w(wcols(w_ba, 0, D, cp * 256, (cp + 1) * 256))
                        for ch in range(2):
                            c = cp * 2 + ch
                            i2 = c % 2
                            bg = bank()
                            mmgroup(bg, ps[:, bg, :], [(wg[:, kc, ch * 128:(ch + 1) * 128], A[:, kc, :]) for kc in range(32)], [TW[s1], TA])
                            bp = bank()
                            mmgroup(bp, ps[:, bp, :], [(wp[:, kc, ch * 128:(ch + 1) * 128], Bv[:, kc, :]) for kc in range(32)], [TW[s2], TB])
                            act(sg[i2], ps[:, bg, :], AF.Sigmoid, [TPS[bg], TC], [Tsg[i2]], bias=pv(V_BGA, c))
                            tt(ma[i2], sg[i2], ps[:, bp, :], ALU.mult, [Tsg[i2], TPS[bp]], [Tma[i2]])
                            P.dma('sp', lambda e, c=c, i2=i2, ma=ma: e.dma_start(out=mixa[c], in_=ma[i2]), [Tma[i2]], [T_MIXA])

                chk(6)
                ov_reset()
                XL = alloc([128, 2, 516], F32)
                TXL = [Tile(), Tile()]
                XC = alloc([128, 2, NB], F32)
                TXC = [Tile(), Tile()]
                XCb = alloc([128, 2, NB], BF16)
                TXCb = Tile()
                tl = [[alloc([128, NB], F32) for _ in range(4)] for _ in range(2)]
                Ttl = [[Tile() for _ in range(4)] for _ in range(2)]
                if blk == 0:
                    tsc(HC, HC, FLAG[:, 0:1], None, ALU.mult, None, [THC, TC], [THC])
                for lb in range(16):
                    s1, wx = load_w(wcols(w_in, 0, D, 6144 + lb * 256, 6144 + (lb + 1) * 256))
                    sa_, wa = load_w(w_lru_a[lb].rearrange("(ic p) j -> p ic j", p=128))
                    si_, wi = load_w(w_lru_i[lb].rearrange("(ic p) j -> p ic j", p=128))
                    for ch in range(2):
                        c = lb * 2 + ch
                        b = bank()
                        mmgroup(b, ps[:, b, :], [(wx[:, kc, ch * 128:(ch + 1) * 128], A[:, kc, :]) for kc in range(32)], [TW[s1], TA])
                        vcopy(XL[:, ch, 0:3], XLT[:, c, :], [TXLT], [TXL[ch]])
                        vcopy(XL[:, ch, 3:515], ps[:, b, :], [TPS[b]], [TXL[ch]])
                        vcopy(XLT[:, c, :], XL[:, ch, 512:515], [TXL[ch]], [TXLT])
                        tsc(XC[:, ch, :], XL[:, ch, 0:512], pv(V_CW0 + 0, c), pv(V_CB, c), ALU.mult, ALU.add, [TXL[ch], TC], [TXC[ch]])
                        for j in range(1, 4):
                            stt(XC[:, ch, :], XL[:, ch, j:j + 512], pv(V_CW0 + j, c), XC[:, ch, :], ALU.mult, ALU.add, [TXL[ch], TC, TXC[ch]], [TXC[ch]])
                        act(XCb[:, ch, :], XC[:, ch, :], AF.Identity, [TXC[ch]], [TXCb])
                    if not pre:
                        s2, wy = load_w(wcols(w_in, 0, D, 10240 + lb * 256, 10240 + (lb + 1) * 256))
                    for ch in range(2):
                        c = lb * 2 + ch
                        R, IG, AA, A2 = tl[ch]
                        TR, TIG, TAA, TA2 = Ttl[ch]
                        U, S_, TU, TS_ = A2, AA, TA2, TAA
                        ba = bank()
                        mmgroup(ba, ps[:, ba, :], [(wa[:, ic, ch * 128:(ch + 1) * 128], XCb[:, ic, :]) for ic in range(2)], [TW[sa_], TXCb])
                        bi = bank()
                        mmgroup(bi, ps[:, bi, :], [(wi[:, ic, ch * 128:(ch + 1) * 128], XCb[:, ic, :]) for ic in range(2)], [TW[si_], TXCb])
                        act(R, ps[:, ba, :], AF.Sigmoid, [TPS[ba], TC], [TR], bias=pv(V_BLA, c))
                        act(IG, ps[:, bi, :], AF.Sigmoid, [TPS[bi], TC], [TIG], bias=pv(V_BLI, c))
                        act(AA, R, AF.Exp, [TR, TC], [TAA], scale=DV[:, 0, c:c + 1])
                        act(A2, R, AF.Exp, [TR, TC], [TA2], scale=DV[:, 1, c:c + 1])
                        tsc(A2, A2, -1.0, 1.0, ALU.mult, ALU.add, [TA2], [TA2])
                        act(A2, A2, AF.Sqrt, [TA2], [TA2])
                        tt(IG, IG, A2, ALU.mult, [TIG, TA2], [TIG])
                        tt(IG, IG, XC[:, ch, :], ALU.mult, [TIG, TXC[ch]], [TIG])
                        P.op('dve', lambda e, R=R, AA=AA, IG=IG, c=c: e.tensor_tensor_scan(out=R, data0=AA, data1=IG, initial=HC[:, c:c + 1], op0=ALU.mult, op1=ALU.add),
                             [TAA, TIG, THC], [TR])
                        vcopy(HC[:, c:c + 1], R[:, NB - 1:NB], [TR], [THC])
                        if not pre:
                            by = bank()
                            mmgroup(by, ps[:, by, :], [(wy[:, kc, ch * 128:(ch + 1) * 128], A[:, kc, :]) for kc in range(32)], [TW[s2], TA])
                            act(U, ps[:, by, :], AF.Square, [TPS[by]], [TU])
                            tsc(U, U, 0.044715, 1.0, ALU.mult, ALU.add, [TU], [TU])
                            tt(U, U, ps[:, by, :], ALU.mult, [TU, TPS[by]], [TU])
                            act(S_, U, AF.Sigmoid, [TU], [TS_], scale=1.5957691216057308)
                            tt(S_, S_, ps[:, by, :], ALU.mult, [TS_, TPS[by]], [TS_])
                            tt(Bv[:, c, :], S_, R, ALU.mult, [TS_, TR], [TB] + TF)
                if pre:
                    continue

                chk(7)
                ov_reset()
                sg = [alloc([128, NB], F32) for _ in range(2)]
                Tsg = [Tile(), Tile()]
                ma = [alloc([128, NB], F32) for _ in range(2)]
                Tma = [Tile(), Tile()]
                mo = [alloc([128, NB], BF16) for _ in range(2)]
                Tmo = [Tile(), Tile()]
                for cp in range(16):
                    s1, wg = load_w(wcols(w_in, 0, D, 18432 + cp * 256, 18432 + (cp + 1) * 256))
                    s2, wp = load_w(wcols(w_bl, 0, D, cp * 256, (cp + 1) * 256))
                    for ch in range(2):
                        c = cp * 2 + ch
                        i2 = c % 2
                        P.dma('sp', lambda e, c=c, i2=i2, ma=ma: e.dma_start(out=ma[i2], in_=mixa[c]), [T_MIXA], [Tma[i2]])
                        bg = bank()
                        mmgroup(bg, ps[:, bg, :], [(wg[:, kc, ch * 128:(ch + 1) * 128], A[:, kc, :]) for kc in range(32)], [TW[s1], TA])
                        bp = bank()
                        mmgroup(bp, ps[:, bp, :], [(wp[:, kc, ch * 128:(ch + 1) * 128], Bv[:, kc, :]) for kc in range(32)], [TW[s2], TB])
                        act(sg[i2], ps[:, bg, :], AF.Sigmoid, [TPS[bg], TC], [Tsg[i2]], bias=pv(V_BGL, c))
                        tt(sg[i2], sg[i2], ps[:, bp, :], ALU.mult, [Tsg[i2], TPS[bp]], [Tsg[i2]])
                        tt(mo[i2], sg[i2], ma[i2], ALU.add, [Tsg[i2], Tma[i2]], [Tmo[i2]])
                        P.dma('sp', lambda e, c=c, i2=i2, mo=mo: e.dma_start(out=mixt[c], in_=mo[i2]), [Tmo[i2]], [T_MIXT])

                chk(8)
                ov_reset()
                xr = [alloc([128, NB], F32) for _ in range(2)]
                Txr = [Tile(), Tile()]
                P.dma('sp', lambda e: e.dma_start(out=A, in_=mixt.rearrange("c p t -> p c t")), [T_MIXT], [TA])
                for cp in range(16):
                    s1, wv = load_w(wcols(w_mo, 0, D, cp * 256, (cp + 1) * 256))
                    for ch in range(2):
                        c = cp * 2 + ch
                        i2 = c % 2
                        P.dma('sp', lambda e, c=c, i2=i2, xr=xr: e.dma_start(out=xr[i2], in_=xres[c]), [T_XRES], [Txr[i2]])
                        b = bank()
                        mmgroup(b, ps[:, b, :], [(wv[:, kc, ch * 128:(ch + 1) * 128], A[:, kc, :]) for kc in range(32)], [TW[s1], TA])
                        stt(Fv[:, c, :], xr[i2], ALPHA, ps[:, b, :], ALU.mult, ALU.add, [Txr[i2], TPS[b]], [TF[c], TB])
                layer_norm(V_L1G, V_L1B, A, True)
                if dbg == 1:
                    dump_dbg()

                chk(9)
                ov_reset()
                QM = alloc([128, 4, NB], BF16)
                TQM = Tile()
                OM = alloc([128, 4, NB], BF16)
                TOM = Tile()
                PTm = [alloc([128, 2, NB], BF16) for _ in range(2)]
                TPTm = [Tile(), Tile()]
                dnm = [alloc([128, NB], F32) for _ in range(2)]
                Tdnm = [Tile(), Tile()]
                for sidx in range(2):
                    s, wv = load_w(wcols(w_mq, 0, D, sidx * 256, (sidx + 1) * 256))
                    for hh in range(2):
                        h = sidx * 2 + hh
                        b = bank()
                        mmgroup(b, ps[:, b, :], [(wv[:, kc, hh * 128:(hh + 1) * 128], A[:, kc, :]) for kc in range(32)], [TW[s], TA])
                        act(QM[:, h, :], ps[:, b, :], AF.Identity, [TPS[b]], [TQM])
                for h in range(4):
                    pi = h % 2
                    for mt in range(2):
                        b = bank()
                        mmgroup(b, ps[:, b, :], [(KMT[:, h, mt * 128:(mt + 1) * 128], QM[:, h, :])], [TC, TQM])
                        act(PTm[pi][:, mt, :], ps[:, b, :], AF.Exp, [TPS[b]], [TPTm[pi]], scale=SCALE)
                    bd = bank()
                    mmgroup(bd, ps[:, bd, :], [(ones_bf, PTm[pi][:, 0, :]), (ones_bf, PTm[pi][:, 1, :])], [TC, TPTm[pi]])
                    bo = bank()
                    mmgroup(bo, ps[:, bo, :], [(VM[:, 0, h * 128:(h + 1) * 128], PTm[pi][:, 0, :]), (VM[:, 1, h * 128:(h + 1) * 128], PTm[pi][:, 1, :])],
                            [TC, TPTm[pi]])
                    P.op('dve', lambda e, pi=pi, bd=bd, dnm=dnm: e.reciprocal(out=dnm[pi], in_=ps[:, bd, :]), [TPS[bd]], [Tdnm[pi]])
                    tt(OM[:, h, :], ps[:, bo, :], dnm[pi], ALU.mult, [TPS[bo], Tdnm[pi]], [TOM])
                for half in range(2):
                    s, wv = load_w(w_mout[:, half * 2048:(half + 1) * 2048].rearrange("(kc p) n -> p kc n", p=128))
                    for cc in range(16):
                        c = half * 16 + cc
                        b = bank()
                        mmgroup(b, ps[:, b, :], [(wv[:, h, cc * 128:(cc + 1) * 128], OM[:, h, :]) for h in range(4)], [TW[s], TOM])
                        stt(Fv[:, c, :], Fv[:, c, :], ALPHA, ps[:, b, :], ALU.mult, ALU.add, [TF[c], TPS[b]], [TF[c]])
                layer_norm(V_L2G, V_L2B, A, True)
                if dbg == 2:
                    dump_dbg()

                chk(10)
                ov_reset()
                L = alloc([128, 32], F32)
                M8 = alloc([128, 8], F32)
                MK = alloc([128, 32], F32)
                NM = alloc([128, 1], F32)
                SS = alloc([128, 1], F32)
                Trt_ = Tile()
                for tti in range(4):
                    b = bank()
                    mmgroup(b, ps[:, b, 0:32], [(Fv[:, c, tti * 128:(tti + 1) * 128], WR[:, c, :]) for c in range(32)], TF + [TC])
                    tt(L, ps[:, b, 0:32], BR, ALU.add, [TPS[b], TC], [Trt_])
                    P.op('dve', lambda e, M8=M8, L=L: e.max(out=M8, in_=L), [Trt_], [Trt_])
                    tsc(MK, L, M8[:, 3:4], None, ALU.is_ge, None, [Trt_], [Trt_])
                    tsc(NM, M8[:, 0:1], -1.0, None, ALU.mult, None, [Trt_], [Trt_])
                    act(L, L, AF.Exp, [Trt_], [Trt_], bias=NM[:, 0:1])
                    tt(L, L, MK, ALU.mult, [Trt_], [Trt_])
                    P.op('dve', lambda e, SS=SS, L=L: e.reduce_sum(out=SS, in_=L, axis=AX.X), [Trt_], [Trt_])
                    P.op('dve', lambda e, SS=SS: e.reciprocal(out=SS, in_=SS), [Trt_], [Trt_])
                    tsc(L, L, SS[:, 0:1], None, ALU.mult, None, [Trt_], [Trt_])
                    b2 = bank()
                    transp(b2, ps[0:32, b2, 0:128], L, ident_f, [Trt_, TC])
                    vcopy(CWT[:, tti * 128:(tti + 1) * 128], ps[0:32, b2, 0:128], [TPS[b2]], [TCWT])
                P.dma('sp', lambda e: e.dma_start(out=cwtd, in_=CWT), [TCWT], [T_CWTD])
                for c in range(32):
                    P.dma('sp', lambda e, c=c: e.dma_start(out=x2res[c], in_=Fv[:, c, :]), [TF[c]], [T_X2RES])

                chk(11)
                ov_reset()
                GLU = alloc([128, 12, NB], BF16)
                TGLU = Tile()
                CWB = [alloc([128, NB], F32) for _ in range(2)]
                TCWB = [Tile(), Tile()]
                et = [[alloc([128, NB], F32) for _ in range(3)] for _ in range(2)]
                Tet = [[Tile() for _ in range(3)] for _ in range(2)]
                for ex in range(nexp):
                    ci = ex % 2
                    P.dma('sp', lambda e, ex=ex, ci=ci, CWB=CWB: e.dma_start(out=CWB[ci], in_=cwtd[ex:ex + 1, :].partition_broadcast(128)), [T_CWTD], [TCWB[ci]])
                    for sidx in range(6):
                        s1, wg = load_w(wcols(w_gu[ex], 0, D, sidx * 256, (sidx + 1) * 256))
                        s2, wu = load_w(wcols(w_gu[ex], 0, D, 1536 + sidx * 256, 1536 + (sidx + 1) * 256))
                        for ch in range(2):
                            f = sidx * 2 + ch
                            i2 = f % 2
                            G, SG_, U1 = et[i2]
                            TG, TSG, TU1 = Tet[i2]
                            bg = bank()
                            mmgroup(bg, ps[:, bg, :], [(wg[:, kc, ch * 128:(ch + 1) * 128], A[:, kc, :]) for kc in range(32)], [TW[s1], TA])
                            bu = bank()
                            mmgroup(bu, ps[:, bu, :], [(wu[:, kc, ch * 128:(ch + 1) * 128], A[:, kc, :]) for kc in range(32)], [TW[s2], TA])
                            tsc(G, ps[:, bg, :], BGU[:, ex * 24 + f:ex * 24 + f + 1], 7.0, ALU.add, ALU.min, [TPS[bg], TC], [TG])
                            act(SG_, G, AF.Sigmoid, [TG], [TSG], scale=1.702)
                            tsc(U1, ps[:, bu, :], BGU[:, ex * 24 + 12 + f:ex * 24 + 12 + f + 1], 7.0, ALU.add, ALU.min, [TPS[bu], TC], [TU1])
                            tsc(U1, U1, -7.0, 1.0, ALU.max, ALU.add, [TU1], [TU1])
                            tt(G, G, SG_, ALU.mult, [TG, TSG], [TG])
                            tt(G, G, U1, ALU.mult, [TG, TU1], [TG])
                            tt(GLU[:, f, :], G, CWB[ci], ALU.mult, [TG, TCWB[ci]], [TGLU])
                    for sidx in range(8):
                        s1, wd = load_w(w_dn[ex][:, sidx * 512:(sidx + 1) * 512].rearrange("(kc p) n -> p kc n", p=128))
                        for ch in range(4):
                            c = sidx * 4 + ch
                            b = bank()
                            mmgroup(b, ps[:, b, :], [(wd[:, fc, ch * 128:(ch + 1) * 128], GLU[:, fc, :]) for fc in range(12)], [TW[s1], TGLU])
                            if ex == 0:
                                vcopy(Fv[:, c, :], ps[:, b, :], [TPS[b]], [TF[c]])
                            else:
                                tt(Fv[:, c, :], Fv[:, c, :], ps[:, b, :], ALU.add, [TF[c], TPS[b]], [TF[c]])

                chk(12)
                ov_reset()
                xr = [alloc([128, NB], F32) for _ in range(2)]
                Txr = [Tile(), Tile()]
                bdn = [alloc([32, 128], F32) for _ in range(2)]
                Tbdn = [Tile(), Tile()]
                for c in range(32):
                    i2 = c % 2
                    P.dma('sp', lambda e, c=c, i2=i2, xr=xr: e.dma_start(out=xr[i2], in_=x2res[c]), [T_X2RES], [Txr[i2]])
                    P.dma('sp', lambda e, c=c, i2=i2, bdn=bdn: e.dma_start(out=bdn[i2], in_=b_dn[:, c * 128:(c + 1) * 128]), [], [Tbdn[i2]])
                    b = bank()
                    mmgroup(b, ps[:, b, :], [(bdn[i2], CWT)], [Tbdn[i2], TCWT])
                    stt(Fv[:, c, :], xr[i2], ALPHA, Fv[:, c, :], ALU.mult, ALU.add, [Txr[i2], TF[c]], [TF[c]])
                    tt(Fv[:, c, :], Fv[:, c, :], ps[:, b, :], ALU.add, [TF[c], TPS[b]], [TF[c]])
                layer_norm(V_L3G, V_L3B, None, False)
                if dbg == 3:
                    dump_dbg()
                ov_reset()
                ost = [alloc([128, 2048], F32) for _ in range(2)]
                Tost = [Tile(), Tile()]
                oi = 0
                for tti in range(4):
                    for hf in range(2):
                        s = oi % 2
                        oi += 1
                        for g in range(4):
                            b = bank()
                            for j in range(4):
                                c = hf * 16 + g * 4 + j
                                transp(b, ps[:, b, j * 128:(j + 1) * 128], Fv[:, c, tti * 128:(tti + 1) * 128], ident_f, [TF[c], TC])
                            if g % 2 == 0:
                                vcopy(ost[s][:, g * 512:(g + 1) * 512], ps[:, b, :], [TPS[b]], [Tost[s]])
                            else:
                                act(ost[s][:, g * 512:(g + 1) * 512], ps[:, b, :], AF.Identity, [TPS[b]], [Tost[s]])
                        orow = NB * blk + tti * 128
                        P.dma('sp', lambda e, s=s, orow=orow, hf=hf, ost=ost: e.dma_start(out=out[orow:orow + 128, hf * 2048:(hf + 1) * 2048], in_=ost[s]),
                              [Tost[s]], [T_OUT])


        try:
            _body()
        except _Stop:
            pass
        P.fence()

        import contextlib
        with contextlib.ExitStack() as es:
            S = {}
            for e in ENGS:
                S[e] = es.enter_context(nc.semaphore("s_" + e))
            for key in P.dcnt:
                S[key] = es.enter_context(nc.semaphore("d_%s%d" % key))
            block = es.enter_context(nc.Block())

            def run(name, eng):
                for fn, waits, inck, incv in P.q[name]:
                    for k, v in waits:
                        eng.wait_ge(S[k], v)
                    if fn is not None:
                        ins = fn(eng)
                        ins.then_inc(S[inck], incv)

            @block.tensor
            def _(e):
                run('pe', e)

            @block.scalar
            def _(e):
                run('act', e)

            @block.vector
            def _(e):
                run('dve', e)

            @block.gpsimd
            def _(e):
                run('pool', e)

            @block.sync
            def _(e):
                run('sp', e)
    nc._decl = decl
    nc._P = P
    return nc


def _consts():
    ident = np.eye(128, dtype=np.float32)
    rotm = np.zeros((32, 32), np.float32)
    for m in range(16):
        rotm[m + 16, m] = -1.0
    for m in range(16, 32):
        rotm[m - 16, m] = 1.0
    invf = (1.0 / (500000.0 ** (np.arange(0, 32, 2, dtype=np.float32) / 32.0))).astype(np.float32)
    invf = (np.concatenate([invf, invf]) / np.float32(2.0 * math.pi)).astype(np.float32).reshape(32, 1)
    s = np.arange(128)[:, None]
    i = np.arange(128)[None, :]
    mprev = np.where(s > i, 0.0, NEG).astype(np.float32)
    mcur = np.where(s <= i, 0.0, NEG).astype(np.float32)
    mband = np.tile(mprev, (1, 4))
    mcur4 = np.tile(mcur, (1, 4))
    mnone = np.full((128, 512), NEG, np.float32)
    return ident, rotm, invf, mband, mcur4, mnone


def make_in_maps(inp):
    ident, rotm, invf, mband, mcur4, mnone = _consts()
    x = np.asarray(inp['x'])
    mem = np.asarray(inp['mem'])
    pos = np.asarray(inp['positions'])
    vec_list = [inp['b_gate'][0, 0], inp['b_gate'][0, 1], inp['conv_w'][0, 0], inp['conv_w'][0, 1], inp['conv_w'][0, 2],
                inp['conv_w'][0, 3], inp['conv_b'][0], inp['b_lru_a'][0].reshape(-1), inp['b_lru_i'][0].reshape(-1),
                inp['lru_lambda'][0], inp['ln1_g'][0], inp['ln1_b'][0], inp['ln2_g'][0], inp['ln2_b'][0], inp['ln3_g'][0], inp['ln3_b'][0]]
    vecs = np.ascontiguousarray(np.stack([np.asarray(v, np.float32).reshape(32, 128) for v in vec_list]).reshape(512, 128))
    shared = {
        "ident": ident, "rotm": rotm, "invf": invf,
        "w_in": np.ascontiguousarray(inp['w_in'][0]), "vecs": vecs,
        "sinks": np.ascontiguousarray(inp['attn_sinks'][0].reshape(1, 32)),
        "w_lru_a": np.ascontiguousarray(inp['w_lru_a'][0]), "w_lru_i": np.ascontiguousarray(inp['w_lru_i'][0]),
        "w_ba": np.ascontiguousarray(inp['w_branch_attn'][0]), "w_bl": np.ascontiguousarray(inp['w_branch_lru'][0]),
        "w_mo": np.ascontiguousarray(inp['w_mix_out'][0]), "w_mq": np.ascontiguousarray(inp['w_mem_q'][0]),
        "w_mkv": np.ascontiguousarray(inp['w_mem_kv'][0]), "w_mout": np.ascontiguousarray(inp['w_mem_o'][0]),
        "w_r": np.ascontiguousarray(inp['w_router'][0]), "b_r": np.ascontiguousarray(inp['b_router'][0].reshape(1, 32)),
        "w_gu": np.ascontiguousarray(inp['w_gate_up'][0]), "b_gu": np.ascontiguousarray(inp['b_gate_up'][0].reshape(768, 128)),
        "w_dn": np.ascontiguousarray(inp['w_down'][0]), "b_dn": np.ascontiguousarray(inp['b_down'][0]),
    }
    maps = []
    for core in range(8):
        b, half = core // 2, core % 2
        m = dict(shared)
        if half == 0:
            xcore = np.concatenate([np.zeros((2048, D), np.float32), x[b, 0:2048]], axis=0)
            pc = np.concatenate([np.zeros((128,), np.int32), pos[b, 0:2048].astype(np.int32)])
            mfirst = mnone
            fl = np.zeros((128, 1), np.float32)
        else:
            xcore = np.ascontiguousarray(x[b])
            pc = np.ascontiguousarray(pos[b, 1920:4096].astype(np.int32))
            mfirst = mband
            fl = np.ones((128, 1), np.float32)
        m["xc"] = np.ascontiguousarray(xcore)
        m["memc"] = np.ascontiguousarray(mem[b])
        m["posc"] = pc.reshape(1, 2176)
        m["flag"] = fl
        m["masks"] = np.ascontiguousarray(np.stack([mfirst, mband, mcur4]))
        maps.append(m)
    return maps


_NC_CACHE = {}


def kernel(**inputs):
    inp = {k: np.asarray(v) for k, v in inputs.items()}
    if "nc" not in _NC_CACHE:
        _NC_CACHE["nc"] = build_program()
    nc = _NC_CACHE["nc"]
    maps = make_in_maps(inp)
    res = run_bass_kernel_spmd(nc, maps, core_ids=list(range(8)))
    outp = np.empty((4, 4096, D), np.float32)
    for core in range(8):
        b, half = core // 2, core % 2
        outp[b, half * 2048:(half + 1) * 2048] = res.results[core]["out"]
    return outp
```
